# Optimizing a Trainium2 kernel written in Bass

```python
import math
import jax
import jax.numpy as jnp
from jax import lax
import numpy as np

D_MODEL = 1024
BATCH = 8
SEQ = 2048
DEPTH = 1

GRID_W = 64
CTX_LEN = 256
NORM_EPS = 1e-6
N_MOD = 6

NA_HEADS = 16
NA_HEAD_DIM = 64
NA_WIDTH = NA_HEADS * NA_HEAD_DIM
NA_KH = 8
NA_KW = 16
ROPE_BASE = 10000.0

SSD_WIDTH = 2 * D_MODEL
SSD_HEAD_DIM = 64
SSD_HEADS = SSD_WIDTH // SSD_HEAD_DIM
SSD_GROUPS = 4
SSD_STATE = 128
SSD_CONV = 5
SSD_CHUNK = 128
SSD_XBC = SSD_WIDTH + 2 * SSD_GROUPS * SSD_STATE

N_EXPERTS = 16
EXPERT_FF = 2048
EC_CAPACITY_FACTOR = 2

IN_COLS = 3 * NA_WIDTH + SSD_WIDTH + SSD_XBC + 2 * SSD_HEADS + 2 * D_MODEL

kernel_name = 'hybrid_na_ssd_ec_diffusion_block'


def _rmsnorm(x, g):
    xf = x.astype(jnp.float32)
    xf = xf * lax.rsqrt(jnp.mean(xf * xf, axis=-1, keepdims=True) + NORM_EPS)
    return (xf * g.astype(jnp.float32)).astype(x.dtype)


def _modulate(x, g, shift, scale):
    return _rmsnorm(x, g) * (1 + scale) + shift


def _ada(cvec, w_ada, b_ada):
    mod = jax.nn.silu(cvec) @ w_ada + b_ada
    return jnp.split(mod, N_MOD, axis=-1)


def _split_in(p):
    offs = np.cumsum([NA_WIDTH, NA_WIDTH, NA_WIDTH, SSD_WIDTH, SSD_XBC, 2 * SSD_HEADS])
    return jnp.split(p, [int(o) for o in offs], axis=-1)


def _heads(t):
    return t.reshape(t.shape[0], t.shape[1], NA_HEADS, NA_HEAD_DIM)


def _rotate(t, pos):
    half = t.shape[-1] // 2
    inv_freq = ROPE_BASE ** (-jnp.arange(half, dtype=jnp.float32) / half)
    ang = pos.astype(jnp.float32)[:, None] * inv_freq[None, :]
    cos = jnp.cos(ang)[:, None, :].astype(t.dtype)
    sin = jnp.sin(ang)[:, None, :].astype(t.dtype)
    t1, t2 = t[..., :half], t[..., half:]
    return jnp.concatenate([t1 * cos - t2 * sin, t1 * sin + t2 * cos], axis=-1)


def _rope_2d(t, rows, cols):
    da = t.shape[-1] // 2
    return jnp.concatenate([_rotate(t[..., :da], rows), _rotate(t[..., da:], cols)], axis=-1)


def _neighbourhood_attention(q, k, v, k_ctx, v_ctx, rpb):
    Bsz, L, H, Dh = q.shape
    rows = L // GRID_W
    kh = min(NA_KH, rows)
    n_loc = kh * GRID_W
    scale = Dh ** -0.5
    qg = q.reshape(Bsz, rows, GRID_W, H, Dh)
    kg = k.reshape(Bsz, rows, GRID_W, H, Dh)
    vg = v.reshape(Bsz, rows, GRID_W, H, Dh)
    col = jnp.arange(GRID_W)
    col_start = jnp.clip(col - NA_KW // 2, 0, GRID_W - NA_KW)
    col_ok = (col[None, :] >= col_start[:, None]) & (col[None, :] < col_start[:, None] + NA_KW)
    col_idx = jnp.clip(col[None, :] - col[:, None] + NA_KW - 1, 0, 2 * NA_KW - 2)
    rpb_col = rpb[:, :, col_idx]
    mask = jnp.tile(col_ok, (1, kh))

    def row_block(r):
        r0 = jnp.clip(r - kh // 2, 0, rows - kh)
        q_r = lax.dynamic_index_in_dim(qg, r, axis=1, keepdims=False)
        k_r = lax.dynamic_slice_in_dim(kg, r0, kh, axis=1).reshape(Bsz, n_loc, H, Dh)
        v_r = lax.dynamic_slice_in_dim(vg, r0, kh, axis=1).reshape(Bsz, n_loc, H, Dh)
        bias = jnp.take(rpb_col, r0 + jnp.arange(kh) - r + NA_KH - 1, axis=1)
        bias = jnp.transpose(bias, (0, 2, 1, 3)).reshape(H, GRID_W, n_loc).astype(jnp.float32)
        s_loc = jnp.einsum('bqhd,bkhd->bhqk', q_r, k_r).astype(jnp.float32) * scale + bias
        s_loc = jnp.where(mask, s_loc, -jnp.inf)
        s_ctx = jnp.einsum('bqhd,bkhd->bhqk', q_r, k_ctx).astype(jnp.float32) * scale
        p = jax.nn.softmax(jnp.concatenate([s_loc, s_ctx], axis=-1), axis=-1).astype(v.dtype)
        return (jnp.einsum('bhqk,bkhd->bqhd', p[..., :n_loc], v_r)
                + jnp.einsum('bhqk,bkhd->bqhd', p[..., n_loc:], v_ctx))

    out = lax.map(row_block, jnp.arange(rows))
    return jnp.moveaxis(out, 0, 1).reshape(Bsz, L, H * Dh)


def _context_attention(q, k, v):
    Bsz, T, H, Dh = q.shape
    s = jnp.einsum('bqhd,bkhd->bhqk', q, k).astype(jnp.float32) * Dh ** -0.5
    p = jax.nn.softmax(s, axis=-1).astype(v.dtype)
    return jnp.einsum('bhqk,bkhd->bqhd', p, v).reshape(Bsz, T, H * Dh)


def _dwconv(x, w, b):
    nch = x.shape[-1]
    out = lax.conv_general_dilated(x, w.astype(x.dtype)[:, None, :], window_strides=(1,),
                                   padding=[(SSD_CONV // 2, SSD_CONV // 2)],
                                   dimension_numbers=('NWC', 'WIO', 'NWC'),
                                   feature_group_count=nch)
    return out + b


def _ssd_prep(xbc, dt_raw, conv_w, conv_b, dtb_f, dtb_b):
    Bsz, L, _ = xbc.shape
    xbc = jax.nn.silu(_dwconv(xbc, conv_w, conv_b))
    xs, Bm, Cm = jnp.split(xbc, [SSD_WIDTH, SSD_WIDTH + SSD_GROUPS * SSD_STATE], axis=-1)
    xs = xs.reshape(Bsz, L, SSD_HEADS, SSD_HEAD_DIM)
    Bm = Bm.reshape(Bsz, L, SSD_GROUPS, SSD_STATE)
    Cm = Cm.reshape(Bsz, L, SSD_GROUPS, SSD_STATE)
    dt_f = jax.nn.softplus(dt_raw[..., :SSD_HEADS] + dtb_f)
    dt_b = jax.nn.softplus(dt_raw[..., SSD_HEADS:] + dtb_b)
    return xs, Bm, Cm, dt_f, dt_b


def _ssd_scan(xs, dt, A, Bm, Cm, h0, return_y):
    Bsz, L, H, P = xs.shape
    nc = L // SSD_CHUNK
    R = H // SSD_GROUPS
    x = (xs * dt[..., None]).reshape(Bsz, nc, SSD_CHUNK, SSD_GROUPS, R, P)
    a = (dt * A).astype(jnp.float32).reshape(Bsz, nc, SSD_CHUNK, SSD_GROUPS, R)
    a_cs = jnp.cumsum(jnp.moveaxis(a, 2, -1), axis=-1)
    Bc = Bm.reshape(Bsz, nc, SSD_CHUNK, SSD_GROUPS, SSD_STATE)
    decay_to_end = jnp.exp(a_cs[..., -1:] - a_cs).astype(xs.dtype)
    states = jnp.einsum('bclgn,bcgrl,bclgrp->bcgrpn', Bc, decay_to_end, x)
    chunk_decay = jnp.exp(a_cs[..., -1]).astype(xs.dtype)

    def step(h, inp):
        dec, st = inp
        return dec[..., None, None] * h + st, h

    h_last, h_prev = lax.scan(step, h0.reshape(Bsz, SSD_GROUPS, R, P, SSD_STATE),
                              (jnp.moveaxis(chunk_decay, 1, 0), jnp.moveaxis(states, 1, 0)))
    h_last = h_last.reshape(Bsz, H, P, SSD_STATE)
    if not return_y:
        return None, h_last
    Cc = Cm.reshape(Bsz, nc, SSD_CHUNK, SSD_GROUPS, SSD_STATE)
    lower = jnp.tril(jnp.ones((SSD_CHUNK, SSD_CHUNK), dtype=bool))
    seg = a_cs[..., :, None] - a_cs[..., None, :]
    Lm = jnp.exp(jnp.where(lower, seg, -jnp.inf)).astype(xs.dtype)
    cb = jnp.einsum('bclgn,bcsgn->bcgls', Cc, Bc)
    y_diag = jnp.einsum('bcgls,bcgrls,bcsgrp->bclgrp', cb, Lm, x)
    y_off = jnp.einsum('bclgn,bcgrpn,bcgrl->bclgrp', Cc, jnp.moveaxis(h_prev, 0, 1),
                       jnp.exp(a_cs).astype(xs.dtype))
    return (y_diag + y_off).reshape(Bsz, L, H, P), h_last


def _bidir_ssd(xs, Bm, Cm, dt_f, dt_b, A_f, A_b, d_skip, h0_f, h0_b, return_y):
    flip = lambda t: jnp.flip(t, axis=1)
    y_f, h_f = _ssd_scan(xs, dt_f, A_f, Bm, Cm, h0_f, return_y)
    y_b, h_b = _ssd_scan(flip(xs), flip(dt_b), A_b, flip(Bm), flip(Cm), h0_b, return_y)
    if not return_y:
        return None, h_f, h_b
    return y_f + flip(y_b) + d_skip[:, None] * xs, h_f, h_b


def _ssd_gated_norm(y, z, g):
    Bsz, L = y.shape[:2]
    u = (y.reshape(Bsz, L, SSD_WIDTH) * jax.nn.silu(z)).astype(jnp.float32)
    u = u.reshape(Bsz, L, SSD_GROUPS, SSD_WIDTH // SSD_GROUPS)
    u = u * lax.rsqrt(jnp.mean(u * u, axis=-1, keepdims=True) + NORM_EPS)
    return (u.reshape(Bsz, L, SSD_WIDTH) * g.astype(jnp.float32)).astype(y.dtype)


def _merge(o_na, y_ssd, gates, w_br_na, w_br_ssd, w_out):
    g_na, g_ssd = jnp.split(gates, 2, axis=-1)
    u = jax.nn.sigmoid(g_na) * (o_na @ w_br_na) + jax.nn.sigmoid(g_ssd) * (y_ssd @ w_br_ssd)
    return u @ w_out


def _token_mixer(h, hc, w_in, na_rpb, conv_w, conv_b, a_log_f, a_log_b, dtb_f, dtb_b, d_skip,
                 ssd_norm, w_br_na, w_br_ssd, w_out, update_ctx):
    Bsz, L, _ = h.shape
    q, k, v, z, xbc, dt_raw, gates = _split_in(h @ w_in)
    qc, kc, vc, zc, xbcc, dtc, gates_c = _split_in(hc @ w_in)
    pos = jnp.arange(L)
    rows, cols = pos // GRID_W, pos % GRID_W
    o_na = _neighbourhood_attention(_rope_2d(_heads(q), rows, cols), _rope_2d(_heads(k), rows, cols),
                                    _heads(v), _heads(kc), _heads(vc), na_rpb)
    A_f, A_b = -jnp.exp(a_log_f), -jnp.exp(a_log_b)
    h0 = jnp.zeros((Bsz, SSD_HEADS, SSD_HEAD_DIM, SSD_STATE), h.dtype)
    y_c, hf_c, hb_c = _bidir_ssd(*_ssd_prep(xbcc, dtc, conv_w, conv_b, dtb_f, dtb_b),
                                 A_f, A_b, d_skip, h0, h0, update_ctx)
    y, _, _ = _bidir_ssd(*_ssd_prep(xbc, dt_raw, conv_w, conv_b, dtb_f, dtb_b),
                         A_f, A_b, d_skip, hf_c, hb_c, True)
    mix = _merge(o_na, _ssd_gated_norm(y, z, ssd_norm), gates, w_br_na, w_br_ssd, w_out)
    if not update_ctx:
        return mix, None
    o_na_c = _context_attention(_heads(qc), _heads(kc), _heads(vc))
    mix_c = _merge(o_na_c, _ssd_gated_norm(y_c, zc, ssd_norm), gates_c, w_br_na, w_br_ssd, w_out)
    return mix, mix_c


def _ec_moe(h, w_router, w_eg, w_eu, w_ed):
    Bsz, T, _ = h.shape
    cap = EC_CAPACITY_FACTOR * T // N_EXPERTS
    aff = jax.nn.softmax((h @ w_router).astype(jnp.float32), axis=-1)
    gate, idx = lax.top_k(jnp.transpose(aff, (0, 2, 1)), cap)
    b_idx = jnp.arange(Bsz)[:, None, None]
    xg = h[b_idx, idx]
    hid = jax.nn.silu(jnp.einsum('becd,edf->becf', xg, w_eg)) * jnp.einsum('becd,edf->becf', xg, w_eu)
    yo = jnp.einsum('becf,efd->becd', hid, w_ed) * gate[..., None].astype(h.dtype)
    return jnp.zeros_like(h).at[b_idx, idx].add(yo)


def _layer(x, ctx, c, c_ctx, w_ada, b_ada, n_pre_mix, n_post_mix, n_pre_ffn, n_post_ffn, w_in, na_rpb,
           conv_w, conv_b, a_log_f, a_log_b, dtb_f, dtb_b, d_skip, ssd_norm, w_br_na, w_br_ssd, w_out,
           w_router, w_eg, w_eu, w_ed, update_ctx):
    sh1, sc1, ga1, sh2, sc2, ga2 = _ada(c[:, None, :], w_ada, b_ada)
    csh1, csc1, cga1, csh2, csc2, cga2 = _ada(c_ctx, w_ada, b_ada)
    mix, mix_c = _token_mixer(_modulate(x, n_pre_mix, sh1, sc1), _modulate(ctx, n_pre_mix, csh1, csc1),
                              w_in, na_rpb, conv_w, conv_b, a_log_f, a_log_b, dtb_f, dtb_b, d_skip,
                              ssd_norm, w_br_na, w_br_ssd, w_out, update_ctx)
    x = x + ga1 * _rmsnorm(mix, n_post_mix)
    x = x + ga2 * _rmsnorm(_ec_moe(_modulate(x, n_pre_ffn, sh2, sc2), w_router, w_eg, w_eu, w_ed), n_post_ffn)
    if update_ctx:
        ctx = ctx + cga1 * _rmsnorm(mix_c, n_post_mix)
        ctx = ctx + cga2 * _rmsnorm(_ec_moe(_modulate(ctx, n_pre_ffn, csh2, csc2), w_router, w_eg, w_eu, w_ed),
                                    n_post_ffn)
    return x, ctx


def setup_inputs(seed: int = 0) -> dict:
    key = jax.random.key(seed)
    ks = jax.random.split(key, 26)
    f32 = jnp.float32

    def nrm(k, shape, s):
        return jax.random.normal(k, shape, f32) * s

    dt0 = jnp.exp(jax.random.uniform(ks[14], (DEPTH, 2, SSD_HEADS), f32, math.log(1e-3), math.log(1e-1)))
    dt_bias = dt0 + jnp.log(-jnp.expm1(-dt0))
    a_log = jnp.log(jax.random.uniform(ks[15], (DEPTH, 2, SSD_HEADS), f32, 1.0, 16.0))
    return {
        'x': nrm(ks[0], (BATCH, SEQ, D_MODEL), 1.0),
        'c': nrm(ks[1], (BATCH, D_MODEL), 1.0),
        'ctx': nrm(ks[2], (BATCH, CTX_LEN, D_MODEL), 1.0),
        'c_ctx': nrm(ks[3], (D_MODEL,), 1.0),
        'w_ada': nrm(ks[4], (DEPTH, D_MODEL, N_MOD * D_MODEL), 0.5 * D_MODEL ** -0.5),
        'b_ada': nrm(ks[5], (DEPTH, N_MOD * D_MODEL), 0.02),
        'norm_pre_mix': 1.0 + nrm(ks[6], (DEPTH, D_MODEL), 0.02),
        'norm_post_mix': 1.0 + nrm(ks[7], (DEPTH, D_MODEL), 0.02),
        'norm_pre_ffn': 1.0 + nrm(ks[8], (DEPTH, D_MODEL), 0.02),
        'norm_post_ffn': 1.0 + nrm(ks[9], (DEPTH, D_MODEL), 0.02),
        'w_in': nrm(ks[10], (DEPTH, D_MODEL, IN_COLS), D_MODEL ** -0.5),
        'na_rpb': nrm(ks[11], (DEPTH, NA_HEADS, 2 * NA_KH - 1, 2 * NA_KW - 1), 0.1),
        'ssd_conv_w': nrm(ks[12], (DEPTH, SSD_CONV, SSD_XBC), SSD_CONV ** -0.5),
        'ssd_conv_b': nrm(ks[13], (DEPTH, SSD_XBC), 0.02),
        'ssd_a_log_fwd': a_log[:, 0],
        'ssd_a_log_bwd': a_log[:, 1],
        'ssd_dt_bias_fwd': dt_bias[:, 0],
        'ssd_dt_bias_bwd': dt_bias[:, 1],
        'ssd_d_skip': 1.0 + nrm(ks[16], (DEPTH, SSD_HEADS), 0.02),
        'ssd_norm': 1.0 + nrm(ks[17], (DEPTH, SSD_WIDTH), 0.02),
        'w_branch_na': nrm(ks[18], (DEPTH, NA_WIDTH, D_MODEL), NA_WIDTH ** -0.5),
        'w_branch_ssd': nrm(ks[19], (DEPTH, SSD_WIDTH, D_MODEL), SSD_WIDTH ** -0.5),
        'w_out': nrm(ks[20], (DEPTH, D_MODEL, D_MODEL), D_MODEL ** -0.5),
        'w_router': nrm(ks[21], (DEPTH, D_MODEL, N_EXPERTS), D_MODEL ** -0.5),
        'w_exp_gate': nrm(ks[22], (DEPTH, N_EXPERTS, D_MODEL, EXPERT_FF), D_MODEL ** -0.5),
        'w_exp_up': nrm(ks[23], (DEPTH, N_EXPERTS, D_MODEL, EXPERT_FF), D_MODEL ** -0.5),
        'w_exp_down': nrm(ks[24], (DEPTH, N_EXPERTS, EXPERT_FF, D_MODEL), EXPERT_FF ** -0.5),
    }


def reference(x, c, ctx, c_ctx, w_ada, b_ada, norm_pre_mix, norm_post_mix, norm_pre_ffn, norm_post_ffn,
              w_in, na_rpb, ssd_conv_w, ssd_conv_b, ssd_a_log_fwd, ssd_a_log_bwd, ssd_dt_bias_fwd,
              ssd_dt_bias_bwd, ssd_d_skip, ssd_norm, w_branch_na, w_branch_ssd, w_out, w_router,
              w_exp_gate, w_exp_up, w_exp_down):
    for i in range(DEPTH):
        x, ctx = _layer(x, ctx, c, c_ctx, w_ada[i], b_ada[i], norm_pre_mix[i], norm_post_mix[i],
                        norm_pre_ffn[i], norm_post_ffn[i], w_in[i], na_rpb[i], ssd_conv_w[i], ssd_conv_b[i],
                        ssd_a_log_fwd[i], ssd_a_log_bwd[i], ssd_dt_bias_fwd[i], ssd_dt_bias_bwd[i],
                        ssd_d_skip[i], ssd_norm[i], w_branch_na[i], w_branch_ssd[i], w_out[i], w_router[i],
                        w_exp_gate[i], w_exp_up[i], w_exp_down[i], i < DEPTH - 1)
    return x
```

```python
from contextlib import ExitStack
import numpy as np
import concourse.bass as bass
import concourse.mybir as mybir
from concourse.bass_utils import run_bass_kernel_spmd

F32 = mybir.dt.float32
BF16 = mybir.dt.bfloat16
AF = mybir.ActivationFunctionType
ALU = mybir.AluOpType
AX = mybir.AxisListType

D = 1024
T = 2048
NT = 16
CT = 256
EPS = 1e-6
NCOL = 10304
DBG = {}
STOP_AFTER = [99]


class Dep:
    __slots__ = ("w", "r")

    def __init__(self):
        self.w = None
        self.r = {}


class TT:
    def __init__(self, t):
        self.t = t
        self.d = Dep()

    def __getitem__(self, k):
        return self.t[k]


class KB:
    ENG = ("pe", "act", "dve", "pool", "sp")

    def __init__(self, nc, gstack):
        self.nc = nc
        self.g = gstack
        self.stack = gstack
        self.sems = {}
        self.cnt = {}
        for e in self.ENG:
            self.sems[e] = gstack.enter_context(nc.semaphore("s_" + e))
            self.cnt[e] = 0
        self.prog = {e: [] for e in self.ENG}
        self.seen = {e: {} for e in self.ENG}
        self.n = 0

    def sb(self, shape, dtype=F32):
        self.n += 1
        return TT(self.stack.enter_context(self.nc.sbuf_tensor(f"sb{self.n}", list(shape), dtype)))

    def ps(self, shape=(128, 512), dtype=F32):
        self.n += 1
        return TT(self.stack.enter_context(self.nc.psum_tensor(f"ps{self.n}", list(shape), dtype)))

    def _deps(self, lst):
        return [x.d if isinstance(x, TT) else x for x in lst]

    def _waits(self, eng, reads, writes):
        need = {}

        def add(k, v):
            if k == "pe" and eng == "pe":
                return
            if need.get(k, 0) < v:
                need[k] = v

        for d in reads:
            if d.w is not None:
                add(*d.w)
        for d in writes:
            if d.w is not None:
                add(*d.w)
            for k, v in d.r.items():
                add(k, v)
        out = []
        sn = self.seen[eng]
        for k, v in need.items():
            if sn.get(k, 0) < v:
                sn[k] = v
                out.append((k, v))
        return out

    def _mark(self, tok, reads, writes):
        k, v = tok
        for d in reads:
            if d.r.get(k, 0) < v:
                d.r[k] = v
        for d in writes:
            d.w = tok
            d.r = {}

    def op(self, eng, fn, R=(), W=()):
        R = self._deps(R)
        W = self._deps(W)
        waits = self._waits(eng, R, W)
        self.cnt[eng] += 1
        self.prog[eng].append((waits, fn, eng, 1))
        self._mark((eng, self.cnt[eng]), R, W)

    def dma(self, q, out, in_, R, W, semo):
        R = self._deps(R)
        W = self._deps(W)
        waits = self._waits(q, R, W)
        key = ("d", id(semo.d if isinstance(semo, TT) else semo))
        if key not in self.sems:
            self.sems[key] = self.g.enter_context(self.nc.semaphore(f"d{len(self.sems)}"))
            self.cnt[key] = 0
        self.cnt[key] += 16
        self.prog[q].append((waits, lambda e: e.dma_start(out=out, in_=in_), key, 16))
        self._mark((key, self.cnt[key]), R, W)

    def barrier(self):
        allk = [(k, v) for k, v in self.cnt.items() if v > 0]
        for e in self.ENG:
            waits = []
            for k, v in allk:
                if k == e:
                    continue
                if self.seen[e].get(k, 0) < v:
                    self.seen[e][k] = v
                    waits.append((k, v))
            self.prog[e].append((waits, None, None, 0))

    def emit(self):
        def replay(e, name):
            for waits, fn, sk, inc in self.prog[name]:
                for k, v in waits:
                    e.wait_ge(self.sems[k], v)
                if fn is not None:
                    fn(e).then_inc(self.sems[sk], inc)

        with self.nc.Block() as block:
            @block.tensor
            def _(e):
                replay(e, "pe")

            @block.scalar
            def _(e):
                replay(e, "act")

            @block.vector
            def _(e):
                replay(e, "dve")

            @block.gpsimd
            def _(e):
                replay(e, "pool")

            @block.sync
            def _(e):
                replay(e, "sp")


def _rope_tables():
    p = np.arange(128)
    dim = p % 64
    axis = dim // 32
    i = dim % 32
    j = i % 16
    inv = (10000.0 ** (-(j.astype(np.float32)) / np.float32(16))).astype(np.float32)
    t = np.arange(T)
    rows = (t // 64).astype(np.float32)
    cols = (t % 64).astype(np.float32)
    pos = np.where(axis[:, None] == 0, rows[None, :], cols[None, :]).astype(np.float32)
    ang = (pos * inv[:, None]).astype(np.float32)
    cos = np.cos(ang).astype(np.float32)
    sin = np.sin(ang).astype(np.float32)
    sgn = np.where(i < 16, -1.0, 1.0).astype(np.float32)
    return cos, (sin * sgn[:, None]).astype(np.float32)


def _perm64():
    d = np.arange(64)
    i = d % 32
    return np.where(i < 16, d + 16, d - 16)


def _host_prep(inp):
    f = np.float32
    sh = {}
    sh["w_ada"] = np.ascontiguousarray(inp["w_ada"][0])
    sh["b_adaT"] = np.ascontiguousarray(inp["b_ada"][0].reshape(48, 128).T)
    sh["b_ada_bc"] = np.ascontiguousarray(np.broadcast_to(inp["b_ada"][0][2048:6144][None, :], (128, 4096)))
    sh["npmT"] = np.ascontiguousarray(inp["norm_pre_mix"][0].reshape(8, 128).T)
    vec = np.stack([inp["norm_post_mix"][0], inp["norm_pre_ffn"][0], inp["norm_post_ffn"][0]])
    sh["vec_bc"] = np.ascontiguousarray(np.broadcast_to(vec.reshape(1, 3 * 1024), (128, 3 * 1024)))
    w_in = inp["w_in"][0]
    sh["w_in"] = np.ascontiguousarray(w_in)
    perm = _perm64()
    colperm = np.concatenate([h * 64 + perm for h in range(16)])
    sh["w_qkp"] = np.ascontiguousarray(np.concatenate([w_in[:, 0:1024][:, colperm], w_in[:, 1024:2048][:, colperm]], axis=1))
    cos, sin = _rope_tables()
    sh["cosT"] = cos
    sh["sinT"] = sin
    rpb = inp["na_rpb"][0]
    kc = np.arange(64)[:, None]
    qc = np.arange(64)[None, :]
    cidx = np.clip(kc - qc + 15, 0, 30)
    rb = np.zeros((128, 16, 14, 64), f)
    for kp in range(2):
        for dd in range(14):
            rb[kp * 64:(kp + 1) * 64, :, dd, :] = np.transpose(rpb[:, dd + kp, :][:, cidx], (1, 0, 2))
    sh["rb"] = np.ascontiguousarray(rb.reshape(128, 16, 7, 2, 64).transpose(0, 1, 3, 2, 4).reshape(128, 16 * 14 * 64))
    cstart = np.clip(np.arange(64) - 8, 0, 48)[None, :]
    m = ((kc >= cstart) & (kc < cstart + 16)).astype(f)
    sh["mask01"] = np.ascontiguousarray(np.concatenate([m, m], axis=0))
    cw = inp["ssd_conv_w"][0]
    sh["convwT"] = np.ascontiguousarray(np.transpose(cw.reshape(5, 24, 128), (2, 1, 0)).reshape(128, 120))
    sh["convbT"] = np.ascontiguousarray(inp["ssd_conv_b"][0].reshape(24, 128).T)
    dtb = np.concatenate([inp["ssd_dt_bias_fwd"][0], inp["ssd_dt_bias_bwd"][0]])
    alog = np.concatenate([inp["ssd_a_log_fwd"][0], inp["ssd_a_log_bwd"][0]])
    sh["dtb_bc"] = np.ascontiguousarray(np.broadcast_to(dtb[None, :], (128, 64)))
    sh["alog_bc"] = np.ascontiguousarray(np.broadcast_to(alog[None, :], (128, 64)))
    sh["dsk_bc"] = np.ascontiguousarray(np.broadcast_to(inp["ssd_d_skip"][0][None, :], (128, 32)))
    sh["ssdn_bc"] = np.ascontiguousarray(np.broadcast_to(inp["ssd_norm"][0][None, :], (128, 2048)))
    sh["w_bna"] = np.ascontiguousarray(inp["w_branch_na"][0])
    sh["w_bssd"] = np.ascontiguousarray(inp["w_branch_ssd"][0])
    sh["w_out"] = np.ascontiguousarray(inp["w_out"][0])
    sh["w_router"] = np.ascontiguousarray(inp["w_router"][0])
    sh["w_eg"] = np.ascontiguousarray(inp["w_exp_gate"][0])
    sh["w_eu"] = np.ascontiguousarray(inp["w_exp_up"][0])
    sh["w_ed"] = np.ascontiguousarray(inp["w_exp_down"][0])
    sh["ident"] = np.eye(128, dtype=f)
    l = np.arange(128)
    sh["triu"] = (l[:, None] <= l[None, :]).astype(f)
    sh["tril"] = (l[:, None] >= l[None, :]).astype(f)
    sh["iota1"] = np.ascontiguousarray(np.broadcast_to(np.arange(1, 257, dtype=f)[None, :], (128, 256)))
    sh["slotp1"] = np.stack([l + 1, l + 129], axis=1).astype(f)
    sel = np.zeros((16, 16, 128), f)
    for e in range(16):
        sel[e, e, :] = 1.0
    sh["sel16"] = np.ascontiguousarray(sel.reshape(16, 2048))
    return sh


SHAPES = {
    "x": [T, D], "ctx": [CT, D], "cc": [128, 16],
    "w_ada": [D, 6144], "b_adaT": [128, 48], "b_ada_bc": [128, 4096], "npmT": [128, 8], "vec_bc": [128, 3072],
    "w_in": [D, NCOL], "w_qkp": [D, 2048], "cosT": [128, T], "sinT": [128, T], "rb": [128, 16 * 14 * 64],
    "mask01": [128, 64], "convwT": [128, 120], "convbT": [128, 24], "dtb_bc": [128, 64], "alog_bc": [128, 64],
    "dsk_bc": [128, 32], "ssdn_bc": [128, 2048], "w_bna": [D, D], "w_bssd": [2048, D], "w_out": [D, D],
    "w_router": [D, 16], "w_eg": [16, D, 2048], "w_eu": [16, D, 2048], "w_ed": [16, 2048, D],
    "ident": [128, 128], "triu": [128, 128], "tril": [128, 128], "iota1": [128, 256], "slotp1": [128, 2],
    "sel16": [16, 2048],
}


def build_program(dbg=None, stop_after=99, only=None, skip=()):
    dbg = dbg or {}
    nc = bass.Bass("TRN2", target_bir_lowering=False)
    I = {k: nc.dram_tensor(k, v, F32, kind="ExternalInput").ap() for k, v in SHAPES.items() if only is None or k in only}
    OUT = nc.dram_tensor("out", [T, D], F32, kind="ExternalOutput").ap()
    DO = {k: nc.dram_tensor("dbg_" + k, list(v[0]), v[1], kind="ExternalOutput").ap() for k, v in dbg.items()}
    onaT_d = nc.dram_tensor("onaT_d", [8, 128, T], BF16).ap()
    ynT_d = nc.dram_tensor("ynT_d", [16, 128, T], BF16).ap()
    x1_d = nc.dram_tensor("x1_d", [T, D], F32).ap()
    h2_d = nc.dram_tensor("h2_d", [T, D], BF16).ap()

    with ExitStack() as G:
        kb = KB(nc, G)
        op = kb.op
        dma = kb.dma
        dbgsem = TT(None)

        def dump(name, src_ap, R):
            if name in DO:
                dma("sp", DO[name], src_ap, R, [], dbgsem)

        ident16 = kb.sb([128, 128], BF16)
        ident32 = kb.sb([128, 128])
        ones16 = kb.sb([128, 128], BF16)
        ones32 = kb.sb([128, 128])
        hT = kb.sb([128, 8, T], BF16)
        hcT = kb.sb([128, 8, CT], BF16)
        dma("sp", ident32[:], I["ident"], [], [ident32], ident32)
        dma("pool", ident16[:], I["ident"], [], [ident16], ident16)
        op("dve", lambda e: e.memset(ones32[:], 1.0), [], [ones32])
        op("dve", lambda e: e.memset(ones16[:], 1.0), [], [ones16])

        with ExitStack() as P:
            kb.stack = P
            cc = kb.sb([128, 16])
            sc16 = kb.sb([128, 16], BF16)
            badT = kb.sb([128, 48])
            npmT = kb.sb([128, 8])
            dma("sp", cc[:], I["cc"], [], [cc], cc)
            dma("sp", badT[:], I["b_adaT"], [], [badT], badT)
            dma("sp", npmT[:], I["npmT"], [], [npmT], npmT)
            op("act", lambda e: e.activation(out=sc16[:], in_=cc[:], func=AF.Silu), [cc], [sc16])
            wa = [kb.sb([128, 8, 1024], BF16) for _ in range(2)]
            wav = I["w_ada"].rearrange("(k p) n -> p k n", p=128)
            for g in range(2):
                dma("pool", wa[g][:], wav[:, :, g * 1024:(g + 1) * 1024], [], [wa[g]], wa[g])
            mps = kb.ps([128, 512])
            for g in range(2):
                for jj in range(8):
                    j = g * 8 + jj
                    for k in range(8):
                        op("pe", lambda e, g=g, jj=jj, j=j, k=k: e.matmul(
                            mps[:, 2 * j:2 * j + 2], wa[g][:, k, jj * 128:(jj + 1) * 128], sc16[:, 2 * k:2 * k + 2],
                            start=(k == 0), stop=(k == 7)), [wa[g], sc16], [mps])
            modT = kb.sb([128, 16, 2])
            op("dve", lambda e: e.tensor_tensor(
                out=modT[:], in0=mps[:, 0:32].rearrange("p (j w) -> p j w", w=2),
                in1=badT[:, 0:16].unsqueeze(2).to_broadcast([128, 16, 2]), op=ALU.add), [mps, badT], [modT])
            a1T = kb.sb([128, 8, 2])
            op("dve", lambda e: e.tensor_scalar(out=a1T[:], in0=modT[:, 8:16, :], scalar1=1.0, scalar2=None, op0=ALU.add),
               [modT], [a1T])
            op("dve", lambda e: e.tensor_tensor(out=a1T[:], in0=a1T[:], in1=npmT[:].unsqueeze(2).to_broadcast([128, 8, 2]),
                                                op=ALU.mult), [a1T, npmT], [a1T])
            xt = [kb.sb([128, D]) for _ in range(2)]
            junk = kb.sb([128, D], BF16)
            xs16 = [kb.sb([128, D], BF16) for _ in range(2)]
            st = [kb.sb([128, 4]) for _ in range(2)]
            tps = [kb.ps([128, 1024], BF16) for _ in range(2)]
            xv = I["x"].rearrange("(i p) d -> i p d", p=128)
            cv = I["ctx"].rearrange("(i p) d -> i p d", p=128)
            for i in range(NT + 2):
                s = i % 2
                isx = i < NT
                src = xv[i] if isx else cv[i - NT]
                w = 0 if isx else 1
                dst = hT if isx else hcT
                c0 = (i if isx else i - NT) * 128
                dma("sp", xt[s][:], src, [], [xt[s]], xt[s])
                op("act", lambda e, s=s: e.activation(out=junk[:], in_=xt[s][:], func=AF.Square, accum_out=st[s][:, 0:1]),
                   [xt[s]], [junk, st[s]])
                op("dve", lambda e, s=s: e.tensor_scalar(out=st[s][:, 1:2], in0=st[s][:, 0:1], scalar1=1.0 / D, scalar2=EPS,
                                                         op0=ALU.mult, op1=ALU.add), [st[s]], [st[s]])
                op("act", lambda e, s=s: e.sqrt(out=st[s][:, 2:3], in_=st[s][:, 1:2]), [st[s]], [st[s]])
                op("dve", lambda e, s=s: e.reciprocal(out=st[s][:, 3:4], in_=st[s][:, 2:3]), [st[s]], [st[s]])
                op("act", lambda e, s=s: e.activation(out=xs16[s][:], in_=xt[s][:], func=AF.Copy, scale=st[s][:, 3:4]),
                   [xt[s], st[s]], [xs16[s]])
                for k in range(8):
                    op("pe", lambda e, s=s, k=k: e.transpose(tps[s][:, k * 128:(k + 1) * 128], xs16[s][:, k * 128:(k + 1) * 128],
                                                             ident16[:]), [xs16[s], ident16], [tps[s]])
                for k in range(8):
                    eng = "dve" if k % 2 == 0 else "act"
                    if eng == "dve":
                        op("dve", lambda e, s=s, k=k, dst=dst, c0=c0, w=w: e.tensor_scalar(
                            out=dst[:, k, c0:c0 + 128], in0=tps[s][:, k * 128:(k + 1) * 128],
                            scalar1=a1T[:, k, w:w + 1], scalar2=modT[:, k, w:w + 1], op0=ALU.mult, op1=ALU.add),
                           [tps[s], a1T, modT], [dst])
                    else:
                        op("act", lambda e, s=s, k=k, dst=dst, c0=c0, w=w: e.activation(
                            out=dst[:, k, c0:c0 + 128], in_=tps[s][:, k * 128:(k + 1) * 128], func=AF.Identity,
                            bias=modT[:, k, w:w + 1], scale=a1T[:, k, w:w + 1]), [tps[s], a1T, modT], [dst])
            kb.barrier()
        kb.stack = G
        if "hT" in DO:
            dump("hT", hT[:], [hT])
            dump("hcT", hcT[:], [hcT])


        if stop_after >= 2 and 2 not in skip:
          with ExitStack() as P:
            kb.stack = P
            winv = I["w_in"].rearrange("(k p) n -> p k n", p=128)
            triu = kb.sb([128, 128]); tril = kb.sb([128, 128])
            dma("sp", triu[:], I["triu"], [], [triu], triu)
            dma("sp", tril[:], I["tril"], [], [tril], tril)
            convw = kb.sb([128, 24, 5]); convb = kb.sb([128, 24])
            dma("sp", convw[:], I["convwT"].rearrange("p (c j) -> p c j", j=5), [], [convw], convw)
            dma("sp", convb[:], I["convbT"], [], [convb], convb)
            dtb = kb.sb([128, 64]); Aneg = kb.sb([128, 64]); dsk = kb.sb([128, 32]); ssdn = kb.sb([128, 512])
            dma("sp", dtb[:], I["dtb_bc"], [], [dtb], dtb)
            dma("sp", Aneg[:], I["alog_bc"], [], [Aneg], Aneg)
            dma("sp", dsk[:], I["dsk_bc"], [], [dsk], dsk)
            op("act", lambda e: e.activation(out=Aneg[:], in_=Aneg[:], func=AF.Exp), [Aneg], [Aneg])
            op("dve", lambda e: e.tensor_scalar(out=Aneg[:], in0=Aneg[:], scalar1=-1.0, scalar2=None, op0=ALU.mult), [Aneg], [Aneg])
            NTT = NT + 2
            wdt = kb.sb([128, 8, 64], BF16)
            dma("pool", wdt[:], winv[:, :, 8192:8256], [], [wdt], wdt)
            dtA = kb.sb([128, NTT, 64]); aA = kb.sb([128, NTT, 64])
            pin = [kb.ps([128, 512]) for _ in range(2)]
            pdt = pin[0]
            for i in range(NTT):
                src = hT if i < NT else hcT
                c0 = (i if i < NT else i - NT) * 128
                for k in range(8):
                    op("pe", lambda e, i=i, k=k, src=src, c0=c0: e.matmul(pdt[:, (i % 8) * 64:(i % 8) * 64 + 64], src[:, k, c0:c0 + 128],
                                                                         wdt[:, k, :], start=(k == 0), stop=(k == 7)), [src, wdt], [pdt])
                if i % 8 == 7 or i == NTT - 1:
                    i0 = (i // 8) * 8
                    n = i - i0 + 1
                    op("dve", lambda e, i0=i0, n=n: e.tensor_tensor(
                        out=dtA[:, i0:i0 + n, :], in0=pdt[:, 0:n * 64].rearrange("p (i c) -> p i c", c=64),
                        in1=dtb[:].unsqueeze(1).to_broadcast([128, n, 64]), op=ALU.add), [pdt, dtb], [dtA])
            op("act", lambda e: e.activation(out=dtA[:], in_=dtA[:], func=AF.Exp), [dtA], [dtA])
            op("act", lambda e: e.activation(out=dtA[:], in_=dtA[:], func=AF.Ln, bias=1.0), [dtA], [dtA])
            op("dve", lambda e: e.tensor_tensor(out=aA[:], in0=dtA[:], in1=Aneg[:].unsqueeze(1).to_broadcast([128, NTT, 64]),
                                                op=ALU.mult), [dtA, Aneg], [aA])
            dump("dt", dtA[:], [dtA])

            wz = kb.sb([128, 8, 512], BF16); wx = kb.sb([128, 8, 512], BF16)
            wB = kb.sb([128, 8, 128], BF16); wC = kb.sb([128, 8, 128], BF16)
            stages = [kb.sb([128, T + 4], BF16) for _ in range(2)]; acc = kb.sb([128, T]); xsTc = kb.sb([128, T], BF16)
            stage_ctr = [0]
            stage_c = kb.sb([128, CT + 4]); acc_c = kb.sb([128, CT]); xsTc_c = kb.sb([128, CT], BF16)
            BT = kb.sb([128, T], BF16); CTt = kb.sb([128, T], BF16)
            BT_c = kb.sb([128, CT], BF16); CT_c = kb.sb([128, CT], BF16)
            xtok = kb.sb([128, NTT, 512], BF16); Btok = kb.sb([128, NTT, 128], BF16)
            yf = kb.sb([128, NT, 512], BF16); ynT = kb.sb([128, 4, T], BF16)
            acs = kb.sb([128, NTT, 16]); tot = kb.sb([128, NTT, 16]); eacs = kb.sb([128, NTT, 16])
            dte = kb.sb([128, NTT, 16]); etot = kb.sb([128, NTT, 16]); ebias = kb.sb([128, NTT, 16])
            ag = kb.sb([128, NTT, 16]); lng = kb.sb([128, NTT, 16]); dtg = kb.sb([128, NTT, 16])
            Dm = kb.sb([128, 8, 128]); t2 = kb.sb([128, 8, 128]); MT = kb.sb([128, 8, 128], BF16)
            cbm = [kb.sb([128, 128]) for _ in range(2)]
            tmp1 = kb.sb([128, 512]); ysum = kb.sb([128, 512]); xd = kb.sb([128, 512], BF16); dskx = kb.sb([128, 512])
            hst = [kb.sb([128, 512]) for _ in range(2)]; hbf = [kb.sb([128, 512], BF16) for _ in range(2)]
            ub = kb.sb([128, 512]); zs = kb.sb([128, 512]); yn16 = kb.sb([128, 512], BF16); gst = kb.sb([128, 4])
            for stg_ in stages:
                op("dve", lambda e, stg_=stg_: e.memset(stg_[:], 0.0), [], [stg_])
            op("dve", lambda e: e.memset(stage_c[:], 0.0), [], [stage_c])
            ptr32 = kb.ps([128, 512])
            ptr = TT(ptr32[:].bitcast(BF16)); ptr.d = ptr32.d
            pR = kb.ps([128, 1024])
            pY = kb.ps([128, 512]); pO = kb.ps([128, 512]); pS = kb.ps([128, 512])

            def inproj_fm(wt, wcols, dstps, src, c0, n):
                for k in range(8):
                    op("pe", lambda e, k=k: e.matmul(dstps[:, 0:n], wt[:, k, wcols], src[:, k, c0:c0 + n],
                                                     start=(k == 0), stop=(k == 7)), [wt, src], [dstps])

            def conv_chunk(wt, wcols, cc_idx, dstT, dstT_c):
                stage = stages[stage_ctr[0] % 2]
                stage_ctr[0] += 1
                for tb in range(4):
                    pp = pin[tb % 2]
                    inproj_fm(wt, wcols, pp, hT, tb * 512, 512)
                    op("act", lambda e, pp=pp, tb=tb: e.copy(out=stage[:, 2 + tb * 512:2 + (tb + 1) * 512], in_=pp[:]), [pp], [stage])
                pp = pin[0]
                inproj_fm(wt, wcols, pp, hcT, 0, CT)
                op("act", lambda e, pp=pp: e.copy(out=stage_c[:, 2:2 + CT], in_=pp[:, 0:CT]), [pp], [stage_c])
                for (stg, ac, n, dst) in ((stage, acc, T, dstT), (stage_c, acc_c, CT, dstT_c)):
                    op("act", lambda e, stg=stg, ac=ac, n=n: e.activation(out=ac[:], in_=stg[:, 0:n], func=AF.Copy, scale=convw[:, cc_idx, 0:1]),
                       [stg, convw], [ac])
                    for j in range(1, 5):
                        op("dve", lambda e, stg=stg, ac=ac, n=n, j=j: e.scalar_tensor_tensor(
                            out=ac[:], in0=stg[:, j:j + n], scalar=convw[:, cc_idx, j:j + 1], in1=ac[:], op0=ALU.mult, op1=ALU.add),
                           [stg, convw, ac], [ac])
                    op("act", lambda e, ac=ac, dst=dst: e.activation(out=dst[:], in_=ac[:], func=AF.Silu, bias=convb[:, cc_idx:cc_idx + 1]),
                       [ac, convb], [dst])

            def do_group(g):
                dma("pool", wz[:], winv[:, :, 3072 + g * 512:3072 + (g + 1) * 512], [], [wz], wz)
                dma("pool", wx[:], winv[:, :, 5120 + g * 512:5120 + (g + 1) * 512], [], [wx], wx)
                dma("pool", wB[:], winv[:, :, 7168 + g * 128:7168 + (g + 1) * 128], [], [wB], wB)
                dma("pool", wC[:], winv[:, :, 7680 + g * 128:7680 + (g + 1) * 128], [], [wC], wC)
                dma("sp", ssdn[:], I["ssdn_bc"][:, g * 512:(g + 1) * 512], [], [ssdn], ssdn)
                for j in range(4):
                    conv_chunk(wx, slice(j * 128, (j + 1) * 128), g * 4 + j, xsTc, xsTc_c)
                    for i0 in range(0, NTT, 8):
                        n = min(8, NTT - i0)
                        for ii in range(n):
                            i = i0 + ii
                            srcT = xsTc if i < NT else xsTc_c
                            c0 = (i if i < NT else i - NT) * 128
                            op("pe", lambda e, ii=ii, srcT=srcT, c0=c0: e.transpose(ptr[:, ii * 128:(ii + 1) * 128], srcT[:, c0:c0 + 128], ident16[:]),
                               [srcT, ident16], [ptr])
                        op("dve", lambda e, i0=i0, n=n, j=j: e.tensor_copy(out=xtok[:, i0:i0 + n, j * 128:(j + 1) * 128],
                                                                         in_=ptr[:, 0:n * 128].rearrange("p (i c) -> p i c", c=128)), [ptr], [xtok])
                conv_chunk(wB, slice(0, 128), 16 + g, BT, BT_c)
                conv_chunk(wC, slice(0, 128), 20 + g, CTt, CT_c)
                for i0 in range(0, NTT, 8):
                    n = min(8, NTT - i0)
                    for ii in range(n):
                        i = i0 + ii
                        srcT = BT if i < NT else BT_c
                        c0 = (i if i < NT else i - NT) * 128
                        op("pe", lambda e, ii=ii, srcT=srcT, c0=c0: e.transpose(ptr[:, ii * 128:(ii + 1) * 128], srcT[:, c0:c0 + 128], ident16[:]),
                           [srcT, ident16], [ptr])
                    op("dve", lambda e, i0=i0, n=n: e.tensor_copy(out=Btok[:, i0:i0 + n, :],
                                                                 in_=ptr[:, 0:n * 128].rearrange("p (i c) -> p i c", c=128)), [ptr], [Btok])
                if g == 0:
                    dump("xtok", xtok[:], [xtok]); dump("Btok", Btok[:], [Btok]); dump("CT", CTt[:], [CTt])
                for (dst, srcA) in ((ag, aA), (dtg, dtA)):
                    op("dve", lambda e, dst=dst, srcA=srcA: e.tensor_copy(out=dst[:, :, 0:8], in_=srcA[:, :, g * 8:g * 8 + 8]), [srcA], [dst])
                    op("dve", lambda e, dst=dst, srcA=srcA: e.tensor_copy(out=dst[:, :, 8:16], in_=srcA[:, :, 32 + g * 8:32 + g * 8 + 8]), [srcA], [dst])
                op("act", lambda e: e.activation(out=lng[:], in_=dtg[:], func=AF.Ln), [dtg], [lng])
                pc = pin[0]; pt = pin[1]
                for i0 in range(0, NTT, 9):
                    for ii in range(9):
                        i = i0 + ii
                        op("pe", lambda e, i=i, ii=ii: e.matmul(pc[:, ii * 16:ii * 16 + 8], triu[:], ag[:, i, 0:8], start=True, stop=True), [triu, ag], [pc])
                        op("pe", lambda e, i=i, ii=ii: e.matmul(pc[:, ii * 16 + 8:ii * 16 + 16], tril[:], ag[:, i, 8:16], start=True, stop=True), [tril, ag], [pc])
                        op("pe", lambda e, i=i, ii=ii: e.matmul(pt[:, ii * 16:ii * 16 + 16], ones32[:], ag[:, i, :], start=True, stop=True), [ones32, ag], [pt])
                    op("dve", lambda e, i0=i0: e.tensor_copy(out=acs[:, i0:i0 + 9, :], in_=pc[:, 0:144].rearrange("p (i c) -> p i c", c=16)), [pc], [acs])
                    op("dve", lambda e, i0=i0: e.tensor_copy(out=tot[:, i0:i0 + 9, :], in_=pt[:, 0:144].rearrange("p (i c) -> p i c", c=16)), [pt], [tot])
                op("act", lambda e: e.activation(out=eacs[:], in_=acs[:], func=AF.Exp), [acs], [eacs])
                op("act", lambda e: e.activation(out=etot[:], in_=tot[:], func=AF.Exp), [tot], [etot])
                op("dve", lambda e: e.tensor_tensor(out=dte[:], in0=tot[:], in1=acs[:], op=ALU.subtract), [tot, acs], [dte])
                op("act", lambda e: e.activation(out=dte[:], in_=dte[:], func=AF.Exp), [dte], [dte])
                op("dve", lambda e: e.tensor_tensor(out=dte[:], in0=dte[:], in1=dtg[:], op=ALU.mult), [dte, dtg], [dte])
                op("dve", lambda e: e.tensor_tensor(out=ebias[:], in0=lng[:], in1=acs[:], op=ALU.subtract), [lng, acs], [ebias])

                def state_update(tile, dr):
                    cs = slice(dr * 8, dr * 8 + 8)
                    op("pool", lambda e: e.tensor_tensor(out=xd[:].rearrange("p (r c) -> p r c", c=64),
                                                        in0=xtok[:, tile, :].rearrange("p (r c) -> p r c", c=64),
                                                        in1=dte[:, tile, cs].unsqueeze(2).to_broadcast([128, 8, 64]), op=ALU.mult), [xtok, dte], [xd])
                    op("pe", lambda e: e.matmul(pO[:], Btok[:, tile, :], xd[:], start=True, stop=True), [Btok, xd], [pO])
                    op("dve", lambda e: e.tensor_tensor(out=hst[dr][:].rearrange("p (r c) -> p r c", c=64),
                                                        in0=hst[dr][:].rearrange("p (r c) -> p r c", c=64),
                                                        in1=etot[:, tile, cs].unsqueeze(2).to_broadcast([128, 8, 64]), op=ALU.mult), [hst[dr], etot], [hst[dr]])
                    op("dve", lambda e: e.tensor_tensor(out=hst[dr][:], in0=hst[dr][:], in1=pO[:], op=ALU.add), [hst[dr], pO], [hst[dr]])
                    op("act", lambda e: e.copy(out=hbf[dr][:], in_=hst[dr][:]), [hst[dr]], [hbf[dr]])

                for dr in range(2):
                    op("dve", lambda e, dr=dr: e.memset(hst[dr][:], 0.0), [], [hst[dr]])
                    op("dve", lambda e, dr=dr: e.memset(hbf[dr][:], 0.0), [], [hbf[dr]])
                state_update(NT, 0); state_update(NT + 1, 0)
                state_update(NT + 1, 1); state_update(NT, 1)
                if g == 0:
                    dump("hf", hst[0][:], [hst[0]]); dump("hb", hst[1][:], [hst[1]])

                tris = (triu, tril)
                pYs = (pY, pS)

                def prepA(dr, c):
                    cs0 = dr * 8
                    csl = slice(c * 128, (c + 1) * 128)
                    pcb = pin[0]
                    cb = cbm[0]
                    op("pe", lambda e: e.matmul(pcb[:, 0:128], BT[:, csl], CTt[:, csl], start=True, stop=True), [BT, CTt], [pcb])
                    op("dve", lambda e: e.tensor_tensor(out=cb[:], in0=pcb[:, 0:128], in1=tris[dr][:], op=ALU.mult), [pcb, tris[dr]], [cb])
                    op("pool", lambda e: e.tensor_tensor(out=Dm[:], in0=ident32[:].unsqueeze(1).to_broadcast([128, 8, 128]),
                                                        in1=acs[:, c, cs0:cs0 + 8].unsqueeze(2).to_broadcast([128, 8, 128]), op=ALU.mult),
                       [ident32, acs], [Dm])
                    for hh in range(2):
                        op("pe", lambda e, hh=hh: e.matmul(pR[:, hh * 512:(hh + 1) * 512], ones32[:],
                                                           Dm[:, hh * 4:(hh + 1) * 4, :].rearrange("p r l -> p (r l)"), start=True, stop=True),
                           [ones32, Dm], [pR])

                def prepB(dr, c):
                    cs0 = dr * 8
                    cb = cbm[0]
                    py = pYs[dr]
                    for r in range(8):
                        op("act", lambda e, r=r: e.activation(out=t2[:, r, :], in_=pR[:, r * 128:(r + 1) * 128], func=AF.Exp,
                                                              bias=ebias[:, c, cs0 + r:cs0 + r + 1]), [pR, ebias], [t2])
                    op("dve", lambda e: e.scalar_tensor_tensor(out=MT[:], in0=t2[:], scalar=1e30,
                                                               in1=cb[:].unsqueeze(1).to_broadcast([128, 8, 128]), op0=ALU.min, op1=ALU.mult),
                       [t2, cb], [MT])
                    for r in range(8):
                        op("pe", lambda e, r=r: e.matmul(py[:, r * 64:(r + 1) * 64], MT[:, r, :], xtok[:, c, r * 64:(r + 1) * 64],
                                                         start=True, stop=True), [MT, xtok], [py])
                    op("pool", lambda e: e.tensor_tensor(out=xd[:].rearrange("p (r c) -> p r c", c=64),
                                                         in0=xtok[:, c, :].rearrange("p (r c) -> p r c", c=64),
                                                         in1=dte[:, c, cs0:cs0 + 8].unsqueeze(2).to_broadcast([128, 8, 64]), op=ALU.mult), [xtok, dte], [xd])
                    op("pe", lambda e: e.matmul(pSt[dr][:], Btok[:, c, :], xd[:], start=True, stop=True), [Btok, xd], [pSt[dr]])

                def rec(dr, c):
                    cs0 = dr * 8
                    csl = slice(c * 128, (c + 1) * 128)
                    py = pYs[dr]
                    first = (dr == 0 and c < 8) or (dr == 1 and c >= 8)
                    op("pe", lambda e: e.matmul(pO[:], CTt[:, csl], hbf[dr][:], start=True, stop=True), [CTt, hbf[dr]], [pO])
                    op("dve", lambda e: e.tensor_tensor(out=hst[dr][:].rearrange("p (r c) -> p r c", c=64),
                                                        in0=hst[dr][:].rearrange("p (r c) -> p r c", c=64),
                                                        in1=etot[:, c, cs0:cs0 + 8].unsqueeze(2).to_broadcast([128, 8, 64]), op=ALU.mult), [hst[dr], etot], [hst[dr]])
                    op("dve", lambda e: e.tensor_tensor(out=hst[dr][:], in0=hst[dr][:], in1=pSt[dr][:], op=ALU.add), [hst[dr], pSt[dr]], [hst[dr]])
                    op("act", lambda e: e.copy(out=hbf[dr][:], in_=hst[dr][:]), [hst[dr]], [hbf[dr]])
                    op("dve", lambda e: e.tensor_tensor(out=tmp1[:].rearrange("p (r c) -> p r c", c=64),
                                                        in0=pO[:].rearrange("p (r c) -> p r c", c=64),
                                                        in1=eacs[:, c, cs0:cs0 + 8].unsqueeze(2).to_broadcast([128, 8, 64]), op=ALU.mult),
                       [pO, eacs], [tmp1])
                    if first:
                        op("dve", lambda e: e.tensor_tensor(out=yf[:, c, :], in0=tmp1[:], in1=py[:], op=ALU.add), [tmp1, py], [yf])
                    else:
                        op("dve", lambda e: e.tensor_tensor(out=ysum[:], in0=tmp1[:], in1=py[:], op=ALU.add), [tmp1, py], [ysum])
                        op("dve", lambda e: e.tensor_tensor(out=ysum[:], in0=ysum[:], in1=yf[:, c, :], op=ALU.add), [ysum, yf], [ysum])
                        op("pool", lambda e: e.tensor_tensor(out=dskx[:].rearrange("p (r c) -> p r c", c=64),
                                                             in0=xtok[:, c, :].rearrange("p (r c) -> p r c", c=64),
                                                             in1=dsk[:, g * 8:g * 8 + 8].unsqueeze(2).to_broadcast([128, 8, 64]), op=ALU.mult),
                           [xtok, dsk], [dskx])
                        op("dve", lambda e: e.tensor_tensor(out=yf[:, c, :], in0=ysum[:], in1=dskx[:], op=ALU.add), [ysum, dskx], [yf])

                def gate(c):
                    csl = slice(c * 128, (c + 1) * 128)
                    pz = pin[c % 2]
                    for k in range(8):
                        op("pe", lambda e, k=k: e.matmul(pz[:], hT[:, k, csl], wz[:, k, :], start=(k == 0), stop=(k == 7)), [hT, wz], [pz])
                    op("act", lambda e: e.activation(out=zs[:], in_=pz[:], func=AF.Silu), [pz], [zs])
                    op("dve", lambda e: e.tensor_tensor(out=ub[:], in0=yf[:, c, :], in1=zs[:], op=ALU.mult), [yf, zs], [ub])
                    op("act", lambda e: e.activation(out=zs[:], in_=ub[:], func=AF.Square, accum_out=gst[:, 0:1]), [ub], [zs, gst])
                    op("dve", lambda e: e.tensor_scalar(out=gst[:, 1:2], in0=gst[:, 0:1], scalar1=1.0 / 512, scalar2=EPS, op0=ALU.mult, op1=ALU.add), [gst], [gst])
                    op("act", lambda e: e.sqrt(out=gst[:, 2:3], in_=gst[:, 1:2]), [gst], [gst])
                    op("dve", lambda e: e.reciprocal(out=gst[:, 3:4], in_=gst[:, 2:3]), [gst], [gst])
                    op("dve", lambda e: e.scalar_tensor_tensor(out=yn16[:], in0=ub[:], scalar=gst[:, 3:4], in1=ssdn[:],
                                                               op0=ALU.mult, op1=ALU.mult), [ub, gst, ssdn], [yn16])
                    for j in range(4):
                        op("pe", lambda e, j=j: e.transpose(ptr[:, j * 128:(j + 1) * 128], yn16[:, j * 128:(j + 1) * 128], ident16[:]), [yn16, ident16], [ptr])
                    op("act", lambda e: e.copy(out=ynT[:, :, csl], in_=ptr[:, 0:512].rearrange("p (j c) -> p j c", c=128)), [ptr], [ynT])

                pSt = (pin[1], ptr32)
                orders = (list(range(NT)), list(range(NT - 1, -1, -1)))
                for s_ in range(NT + 1):
                    for dr in range(2):
                        if s_ < NT:
                            prepA(dr, orders[dr][s_])
                        if s_ >= 1:
                            rec(dr, orders[dr][s_ - 1])
                        if s_ < NT:
                            prepB(dr, orders[dr][s_])
                for c in range(NT):
                    gate(c)
                dma("sp", ynT_d[g * 4:(g + 1) * 4].rearrange("j p t -> p j t"), ynT[:], [ynT], [], ynT)
                if g == 0:
                    dump("ynT0", ynT[:], [ynT])
            for g in range(4):
                do_group(g)
            kb.barrier()
          kb.stack = G


        if stop_after >= 3 and 3 not in skip:
          with ExitStack() as P:
            kb.stack = P
            winv = I["w_in"].rearrange("(k p) n -> p k n", p=128)
            wqkpv = I["w_qkp"].rearrange("(k p) n -> p k n", p=128)
            cosT = kb.sb([128, T]); sinT = kb.sb([128, T]); mask = kb.sb([128, 64])
            dma("sp", cosT[:], I["cosT"], [], [cosT], cosT)
            dma("sp", sinT[:], I["sinT"], [], [sinT], sinT)
            dma("sp", mask[:], I["mask01"], [], [mask], mask)
            EB = kb.sb([128, 16, 2, 7, 64], BF16)
            rbs = kb.sb([128, 14, 64])
            rbv = I["rb"].rearrange("p (h d c) -> p h d c", h=16, d=14)
            for h in range(16):
                dma("sp", rbs[:], rbv[:, h], [], [rbs], rbs)
                op("act", lambda e: e.activation(out=rbs[:], in_=rbs[:], func=AF.Exp), [rbs], [rbs])
                op("dve", lambda e, h=h: e.tensor_tensor(out=EB[:, h].rearrange("p v u c -> p (v u) c"), in0=rbs[:],
                                                         in1=mask[:].unsqueeze(1).to_broadcast([128, 14, 64]), op=ALU.mult), [rbs, mask], [EB])
            wq = kb.sb([128, 8, 128], BF16); wk = kb.sb([128, 8, 128], BF16); wv = kb.sb([128, 8, 128], BF16)
            wqp = kb.sb([128, 8, 128], BF16); wkp = kb.sb([128, 8, 128], BF16)
            qT = kb.sb([128, T], BF16); kT = kb.sb([128, T], BF16); kcT = kb.sb([128, CT], BF16)
            qz = [kb.sb([128, T], BF16) for _ in range(2)]
            for zz in qz:
                op("dve", lambda e, zz=zz: e.memset(zz[:], 0.0), [], [zz])
            vE = kb.sb([128, 16, 256], BF16); vO = kb.sb([128, 15, 256], BF16); vC = kb.sb([128, 2, 256], BF16)
            for vv in (vE, vO, vC):
                op("dve", lambda e, vv=vv: e.memset(vv[:], 1.0), [], [vv])
            onaT = kb.sb([128, T], BF16)
            r1 = kb.sb([128, 512]); r2 = kb.sb([128, 512])
            PT = [kb.sb([128, 768], BF16) for _ in range(2)]
            rec = [kb.sb([128, 128]) for _ in range(2)]
            bA = kb.ps([128, 512]); bB = kb.ps([128, 512]); bC = kb.ps([128, 512])
            pSs = [kb.ps([128, 1024]) for _ in range(2)]
            pOD = [bA, bB]

            def rope_proj(w, wp, dst):
                for tb in range(4):
                    ts = slice(tb * 512, (tb + 1) * 512)
                    for k in range(8):
                        op("pe", lambda e, k=k, ts=ts: e.matmul(bA[:], w[:, k, :], hT[:, k, ts], start=(k == 0), stop=(k == 7)), [w, hT], [bA])
                    for k in range(8):
                        op("pe", lambda e, k=k, ts=ts: e.matmul(bB[:], wp[:, k, :], hT[:, k, ts], start=(k == 0), stop=(k == 7)), [wp, hT], [bB])
                    op("dve", lambda e, ts=ts: e.tensor_tensor(out=r1[:], in0=bA[:], in1=cosT[:, ts], op=ALU.mult), [bA, cosT], [r1])
                    op("dve", lambda e, ts=ts: e.tensor_tensor(out=r2[:], in0=bB[:], in1=sinT[:, ts], op=ALU.mult), [bB, sinT], [r2])
                    op("dve", lambda e, ts=ts: e.tensor_tensor(out=dst[:, ts], in0=r1[:], in1=r2[:], op=ALU.add), [r1, r2], [dst])

            def v_tiles(dstv, ntile, src, tok0):
                for i0 in range(0, ntile, 4):
                    n = min(4, ntile - i0)
                    for ii in range(n):
                        c0 = tok0 + (i0 + ii) * 128
                        for k in range(8):
                            op("pe", lambda e, k=k, ii=ii, c0=c0: e.matmul(bC[:, ii * 128:(ii + 1) * 128], src[:, k, c0:c0 + 128], wv[:, k, :],
                                                                         start=(k == 0), stop=(k == 7)), [src, wv], [bC])
                    pv = bC[:, 0:n * 128].rearrange("p (i c) -> p i c", c=128)
                    op("act", lambda e, i0=i0, n=n, pv=pv: e.copy(out=dstv[:, i0:i0 + n, 64:192], in_=pv[:, :, 0:128]), [bC], [dstv])

            def row_scores(hp, r):
                s = r % 2
                pS = pSs[s]; pt = PT[s]
                r0 = min(max(r - 4, 0), 24)
                base = r0 - r + 7
                v_, u0 = base % 2, base // 2
                qs = slice(r * 64, (r + 1) * 64)
                for h in range(2):
                    hs = slice(h * 64, (h + 1) * 64)
                    for m in range(6):
                        if m < 4:
                            ks = (r0 + 2 * m) * 64
                            lhs = kT[:, ks:ks + 128]
                        else:
                            lhs = kcT[:, (m - 4) * 128:(m - 3) * 128]
                        o0 = h * 384 + m * 64
                        op("pe", lambda e, lhs=lhs, o0=o0, h=h: e.matmul(pS[:, o0:o0 + 64], lhs, qz[h][:, qs], start=True, stop=True), [kT, kcT, qz[h]], [pS])
                op("act", lambda e: e.activation(out=pt[:, 0:512], in_=pS[:, 0:512], func=AF.Exp, scale=0.125), [pS], [pt])
                op("act", lambda e: e.activation(out=pt[:, 512:768], in_=pS[:, 512:768], func=AF.Exp, scale=0.125), [pS], [pt])
                ptv = pt[:].rearrange("p (h m c) -> p h m c", h=2, m=6)
                op("dve", lambda e: e.tensor_tensor(out=ptv[:, :, 0:4, :], in0=ptv[:, :, 0:4, :], in1=EB[:, 2 * hp:2 * hp + 2, v_, u0:u0 + 4, :], op=ALU.mult),
                   [pt, EB], [pt])

            def row_pv(hp, r):
                s = r % 2
                pt = PT[s]; po = pOD[s]; rc = rec[s]
                r0 = min(max(r - 4, 0), 24)
                qs = slice(r * 64, (r + 1) * 64)
                ptv = pt[:].rearrange("p (h m c) -> p h m c", h=2, m=6)
                for h in range(2):
                    for m in range(6):
                        if m < 4:
                            kr = r0 + 2 * m
                            vt = vE[:, kr // 2, h * 128:(h + 1) * 128] if kr % 2 == 0 else vO[:, (kr - 1) // 2, h * 128:(h + 1) * 128]
                        else:
                            vt = vC[:, m - 4, h * 128:(h + 1) * 128]
                        op("pe", lambda e, vt=vt, h=h, m=m: e.matmul(po[:, h * 64:(h + 1) * 64], vt, ptv[:, h, m, :], start=(m == 0), stop=(m == 5)),
                           [vE, vO, vC, pt], [po])
                op("dve", lambda e: e.reciprocal(out=rc[0:64, 0:64], in_=po[0:64, 0:64]), [po], [rc])
                op("dve", lambda e: e.reciprocal(out=rc[64:128, 64:128], in_=po[64:128, 64:128]), [po], [rc])
                op("dve", lambda e: e.tensor_tensor(out=onaT[0:64, qs], in0=po[64:128, 0:64], in1=rc[0:64, 0:64], op=ALU.mult), [po, rc], [onaT])
                op("dve", lambda e: e.tensor_tensor(out=onaT[64:128, qs], in0=po[0:64, 64:128], in1=rc[64:128, 64:128], op=ALU.mult), [po, rc], [onaT])

            def do_pair(hp):
                c0 = hp * 128
                dma("pool", wq[:], winv[:, :, c0:c0 + 128], [], [wq], wq)
                dma("pool", wk[:], winv[:, :, 1024 + c0:1024 + c0 + 128], [], [wk], wk)
                dma("pool", wv[:], winv[:, :, 2048 + c0:2048 + c0 + 128], [], [wv], wv)
                dma("pool", wqp[:], wqkpv[:, :, c0:c0 + 128], [], [wqp], wqp)
                dma("pool", wkp[:], wqkpv[:, :, 1024 + c0:1024 + c0 + 128], [], [wkp], wkp)
                rope_proj(wq, wqp, qT)
                rope_proj(wk, wkp, kT)
                op("act", lambda e: e.copy(out=qz[0][0:64, :], in_=qT[0:64, :]), [qT], [qz[0]])
                op("act", lambda e: e.copy(out=qz[1][64:128, :], in_=qT[64:128, :]), [qT], [qz[1]])
                for k in range(8):
                    op("pe", lambda e, k=k: e.matmul(bA[:, 0:CT], wk[:, k, :], hcT[:, k, :], start=(k == 0), stop=(k == 7)), [wk, hcT], [bA])
                op("act", lambda e: e.copy(out=kcT[:], in_=bA[:, 0:CT]), [bA], [kcT])
                v_tiles(vE, 16, hT, 0)
                v_tiles(vO, 15, hT, 64)
                v_tiles(vC, 2, hcT, 0)
                row_scores(hp, 0)
                for r in range(32):
                    if r + 1 < 32:
                        row_scores(hp, r + 1)
                    row_pv(hp, r)
                dma("sp", onaT_d[hp], onaT[:], [onaT], [], onaT)
                if hp == 0:
                    dump("qT0", qT[:], [qT]); dump("kT0", kT[:], [kT]); dump("onaT0", onaT[:], [onaT])

            for hp in range(8):
                do_pair(hp)
            kb.barrier()
          kb.stack = G


        kb.stack = G
        modbc = kb.sb([128, 4, 1024])
        affs = kb.sb([128, NT, 16])
        if stop_after >= 4 and 4 not in skip:
          with ExitStack() as P0:
            kb.stack = P0
            cc2 = kb.sb([128, 16]); sc32 = kb.sb([128, 16]); screp = kb.sb([128, 8, 128], BF16)
            vecb = kb.sb([128, 3072]); wa2 = kb.sb([128, 8, 1024], BF16); badb = kb.sb([128, 1024])
            pm = [kb.ps([128, 512]) for _ in range(2)]
            dma("sp", cc2[:], I["cc"], [], [cc2], cc2)
            dma("sp", vecb[:], I["vec_bc"], [], [vecb], vecb)
            op("act", lambda e: e.activation(out=sc32[:], in_=cc2[:], func=AF.Silu), [cc2], [sc32])
            op("dve", lambda e: e.tensor_copy(out=screp[:], in_=sc32[:].rearrange("p (k w) -> p k w", w=2)[:, :, 0:1].to_broadcast([128, 8, 128])),
               [sc32], [screp])
            wav2 = I["w_ada"].rearrange("(k p) n -> p k n", p=128)
            for c in range(4):
                dma("pool", wa2[:], wav2[:, :, (c + 2) * 1024:(c + 3) * 1024], [], [wa2], wa2)
                dma("sp", badb[:], I["b_ada_bc"][:, c * 1024:(c + 1) * 1024], [], [badb], badb)
                for hf in range(2):
                    for k in range(8):
                        op("pe", lambda e, k=k, hf=hf: e.matmul(pm[hf][:], screp[:, k, :], wa2[:, k, hf * 512:(hf + 1) * 512], start=(k == 0), stop=(k == 7)),
                           [screp, wa2], [pm[hf]])
                    op("dve", lambda e, c=c, hf=hf: e.tensor_tensor(out=modbc[:, c, hf * 512:(hf + 1) * 512], in0=pm[hf][:], in1=badb[:, hf * 512:(hf + 1) * 512], op=ALU.add),
                       [pm[hf], badb], [modbc])
            op("dve", lambda e: e.tensor_tensor(out=modbc[:, 0, :], in0=modbc[:, 0, :], in1=vecb[:, 0:1024], op=ALU.mult), [modbc, vecb], [modbc])
            op("dve", lambda e: e.scalar_tensor_tensor(out=modbc[:, 2, :], in0=modbc[:, 2, :], scalar=1.0, in1=vecb[:, 1024:2048], op0=ALU.add, op1=ALU.mult),
               [modbc, vecb], [modbc])
            op("dve", lambda e: e.tensor_tensor(out=modbc[:, 3, :], in0=modbc[:, 3, :], in1=vecb[:, 2048:3072], op=ALU.mult), [modbc, vecb], [modbc])
            kb.barrier()
          with ExitStack() as P:
            kb.stack = P
            winv = I["w_in"].rearrange("(k p) n -> p k n", p=128)
            wbnv = I["w_bna"].rearrange("(k p) n -> p k n", p=128)
            wbsv = I["w_bssd"].rearrange("(k p) n -> p k n", p=128)
            wo = kb.sb([128, 8, 1024], BF16)
            dma("pool", wo[:], I["w_out"].rearrange("(k p) n -> p k n", p=128), [], [wo], wo)
            wr = kb.sb([128, 8, 16])
            dma("sp", wr[:], I["w_router"].rearrange("(k p) n -> p k n", p=128), [], [wr], wr)
            onaTs = kb.sb([128, 8, 1024], BF16); ynTs = kb.sb([128, 16, 1024], BF16); uT = kb.sb([128, 8, 1024], BF16)
            wbn = [kb.sb([128, 8, 128], BF16) for _ in range(2)]; wbs = [kb.sb([128, 16, 128], BF16) for _ in range(2)]
            wg1 = [kb.sb([128, 8, 128], BF16) for _ in range(2)]; wg2 = [kb.sb([128, 8, 128], BF16) for _ in range(2)]
            s1 = kb.sb([128, 512]); s2 = kb.sb([128, 512])
            xt4 = kb.sb([128, D]); x1t4 = kb.sb([128, D]); tmpf = kb.sb([128, D]); h2f = kb.sb([128, D]); h2b = kb.sb([128, D], BF16)
            h2T = kb.sb([128, 8, 128]); stt4 = kb.sb([128, 8]); junk4 = kb.sb([128, D], BF16)
            lg = kb.sb([128, 16]); sm = kb.sb([128, 4])
            pA = kb.ps([128, 512]); pB = kb.ps([128, 512]); pG1 = kb.ps([128, 512]); pG2 = kb.ps([128, 512])
            pM = [kb.ps([128, 512]) for _ in range(2)]; pT = [kb.ps([128, 512]) for _ in range(2)]
            xv = I["x"].rearrange("(i p) d -> i p d", p=128)
            x1v = x1_d.rearrange("(i p) d -> i p d", p=128)
            h2v = h2_d.rearrange("(i p) d -> i p d", p=128)

            def merge_block(th, dc, tb, W):
                wbn_, wbs_, wg1_, wg2_ = W
                tsl = slice(tb * 512, (tb + 1) * 512)
                gsl = slice(th * 1024 + tb * 512, th * 1024 + (tb + 1) * 512)
                for k in range(8):
                    op("pe", lambda e, k=k: e.matmul(pA[:], wbn_[:, k, :], onaTs[:, k, tsl], start=(k == 0), stop=(k == 7)), [wbn_, onaTs], [pA])
                for k in range(16):
                    op("pe", lambda e, k=k: e.matmul(pB[:], wbs_[:, k, :], ynTs[:, k, tsl], start=(k == 0), stop=(k == 15)), [wbs_, ynTs], [pB])
                for k in range(8):
                    op("pe", lambda e, k=k: e.matmul(pG1[:], wg1_[:, k, :], hT[:, k, gsl], start=(k == 0), stop=(k == 7)), [wg1_, hT], [pG1])
                for k in range(8):
                    op("pe", lambda e, k=k: e.matmul(pG2[:], wg2_[:, k, :], hT[:, k, gsl], start=(k == 0), stop=(k == 7)), [wg2_, hT], [pG2])
                op("act", lambda e: e.activation(out=s1[:], in_=pG1[:], func=AF.Sigmoid), [pG1], [s1])
                op("act", lambda e: e.activation(out=s2[:], in_=pG2[:], func=AF.Sigmoid), [pG2], [s2])
                op("dve", lambda e: e.tensor_tensor(out=s1[:], in0=s1[:], in1=pA[:], op=ALU.mult), [s1, pA], [s1])
                op("dve", lambda e: e.tensor_tensor(out=s2[:], in0=s2[:], in1=pB[:], op=ALU.mult), [s2, pB], [s2])
                op("dve", lambda e: e.tensor_tensor(out=uT[:, dc, tsl], in0=s1[:], in1=s2[:], op=ALU.add), [s1, s2], [uT])

            def rstd_chain(col):
                op("dve", lambda e: e.tensor_scalar(out=stt4[:, col + 1:col + 2], in0=stt4[:, col:col + 1], scalar1=1.0 / D, scalar2=EPS, op0=ALU.mult, op1=ALU.add), [stt4], [stt4])
                op("act", lambda e: e.sqrt(out=stt4[:, col + 2:col + 3], in_=stt4[:, col + 1:col + 2]), [stt4], [stt4])
                op("dve", lambda e: e.reciprocal(out=stt4[:, col + 3:col + 4], in_=stt4[:, col + 2:col + 3]), [stt4], [stt4])

            def post_tile(th, i):
                gi = th * 8 + i
                isl = slice(i * 128, (i + 1) * 128)
                for hf in range(2):
                    for k in range(8):
                        op("pe", lambda e, k=k, hf=hf: e.matmul(pM[hf][:], uT[:, k, isl], wo[:, k, hf * 512:(hf + 1) * 512], start=(k == 0), stop=(k == 7)), [uT, wo], [pM[hf]])
                dma("sp", xt4[:], xv[gi], [], [xt4], xt4)
                op("act", lambda e: e.activation(out=junk4[:, 0:512], in_=pM[0][:], func=AF.Square, accum_out=stt4[:, 0:1]), [pM[0]], [junk4, stt4])
                op("act", lambda e: e.activation(out=junk4[:, 512:1024], in_=pM[1][:], func=AF.Square, accum_out=stt4[:, 4:5]), [pM[1]], [junk4, stt4])
                op("dve", lambda e: e.tensor_tensor(out=stt4[:, 0:1], in0=stt4[:, 0:1], in1=stt4[:, 4:5], op=ALU.add), [stt4], [stt4])
                rstd_chain(0)
                for hf in range(2):
                    hs = slice(hf * 512, (hf + 1) * 512)
                    op("dve", lambda e, hf=hf, hs=hs: e.scalar_tensor_tensor(out=x1t4[:, hs], in0=pM[hf][:], scalar=stt4[:, 3:4], in1=modbc[:, 0, hs], op0=ALU.mult, op1=ALU.mult),
                       [pM[hf], stt4, modbc], [x1t4])
                op("dve", lambda e: e.tensor_tensor(out=x1t4[:], in0=x1t4[:], in1=xt4[:], op=ALU.add), [x1t4, xt4], [x1t4])
                dma("sp", x1v[gi], x1t4[:], [x1t4], [], x1t4)
                if gi == 0:
                    dump("x1_0", x1t4[:], [x1t4])
                op("act", lambda e: e.activation(out=junk4[:], in_=x1t4[:], func=AF.Square, accum_out=stt4[:, 0:1]), [x1t4], [junk4, stt4])
                rstd_chain(0)
                op("dve", lambda e: e.scalar_tensor_tensor(out=tmpf[:], in0=x1t4[:], scalar=stt4[:, 3:4], in1=modbc[:, 2, :], op0=ALU.mult, op1=ALU.mult), [x1t4, stt4, modbc], [tmpf])
                op("dve", lambda e: e.tensor_tensor(out=h2f[:], in0=tmpf[:], in1=modbc[:, 1, :], op=ALU.add), [tmpf, modbc], [h2f])
                op("act", lambda e: e.copy(out=h2b[:], in_=h2f[:]), [h2f], [h2b])
                dma("sp", h2v[gi], h2b[:], [h2b], [], h2b)
                for k in range(8):
                    op("pe", lambda e, k=k: e.transpose(pT[k // 4][:, (k % 4) * 128:(k % 4 + 1) * 128], h2f[:, k * 128:(k + 1) * 128], ident32[:]), [h2f, ident32], [pT[k // 4]])
                op("act", lambda e: e.copy(out=h2T[:, 0:4, :], in_=pT[0][:].rearrange("p (k c) -> p k c", c=128)), [pT[0]], [h2T])
                op("dve", lambda e: e.tensor_copy(out=h2T[:, 4:8, :], in_=pT[1][:].rearrange("p (k c) -> p k c", c=128)), [pT[1]], [h2T])
                for k in range(8):
                    op("pe", lambda e, k=k: e.matmul(pA[:, 0:16], h2T[:, k, :], wr[:, k, :], start=(k == 0), stop=(k == 7)), [h2T, wr], [pA])
                op("dve", lambda e: e.tensor_copy(out=lg[:], in_=pA[:, 0:16]), [pA], [lg])
                op("dve", lambda e: e.reduce_max(out=sm[:, 0:1], in_=lg[:], axis=AX.X), [lg], [sm])
                op("dve", lambda e: e.tensor_scalar(out=sm[:, 1:2], in0=sm[:, 0:1], scalar1=-1.0, scalar2=None, op0=ALU.mult), [sm], [sm])
                op("act", lambda e: e.activation(out=lg[:], in_=lg[:], func=AF.Exp, bias=sm[:, 1:2], accum_out=sm[:, 2:3]), [lg, sm], [lg, sm])
                op("dve", lambda e: e.reciprocal(out=sm[:, 3:4], in_=sm[:, 2:3]), [sm], [sm])
                op("dve", lambda e: e.tensor_scalar(out=affs[:, gi, :], in0=lg[:], scalar1=sm[:, 3:4], scalar2=None, op0=ALU.mult), [lg, sm], [affs])

            for th in range(2):
                hsl = slice(th * 1024, (th + 1) * 1024)
                dma("sp", onaTs[:], onaT_d[:, :, hsl].rearrange("c p t -> p c t"), [], [onaTs], onaTs)
                dma("sp", ynTs[:], ynT_d[:, :, hsl].rearrange("c p t -> p c t"), [], [ynTs], ynTs)
                for dc in range(8):
                    s_ = dc % 2
                    W = (wbn[s_], wbs[s_], wg1[s_], wg2[s_])
                    dsl = slice(dc * 128, (dc + 1) * 128)
                    dma("pool", W[0][:], wbnv[:, :, dsl], [], [W[0]], W[0])
                    dma("pool", W[1][:], wbsv[:, :, dsl], [], [W[1]], W[1])
                    dma("pool", W[2][:], winv[:, :, 8256 + dc * 128:8256 + (dc + 1) * 128], [], [W[2]], W[2])
                    dma("pool", W[3][:], winv[:, :, 9280 + dc * 128:9280 + (dc + 1) * 128], [], [W[3]], W[3])
                    for tb in range(2):
                        merge_block(th, dc, tb, W)
                for i in range(8):
                    post_tile(th, i)
            dump("affs", affs[:], [affs])
            kb.barrier()
          kb.stack = G

        if stop_after >= 5 and 5 not in skip:
          with ExitStack() as P:
            kb.stack = P
            h2 = kb.sb([128, NT, D], BF16)
            dma("sp", h2[:], h2_d.rearrange("(i p) d -> p i d", p=128), [], [h2], h2)
            yo = kb.sb([128, 32, D], BF16)
            csm16 = kb.sb([16, T], BF16); csmT = kb.sb([128, NT, 16]); affhl = kb.sb([128, NT, 16, 2], BF16)
            iota1 = kb.sb([128, 256]); slotp1 = kb.sb([128, 2]); sel16 = kb.sb([16, 16, 128], BF16)
            dma("sp", iota1[:], I["iota1"], [], [iota1], iota1)
            dma("sp", slotp1[:], I["slotp1"], [], [slotp1], slotp1)
            dma("pool", sel16[:], I["sel16"].rearrange("k (e m) -> k e m", e=16), [], [sel16], sel16)
            with ExitStack() as P1:
                kb.stack = P1
                affT = kb.sb([16, T]); work = kb.sb([16, T]); csb = kb.sb([16, T]); mx8 = kb.sb([16, 8]); onesr = kb.sb([16, T])
                hi32 = kb.sb([128, NT, 16]); lo32 = kb.sb([128, NT, 16])
                pr = [kb.ps([128, 512]) for _ in range(2)]
                op("dve", lambda e: e.tensor_copy(out=affhl[:, :, :, 0], in_=affs[:]), [affs], [affhl])
                op("dve", lambda e: e.tensor_copy(out=hi32[:], in_=affhl[:, :, :, 0]), [affhl], [hi32])
                op("dve", lambda e: e.tensor_tensor(out=lo32[:], in0=affs[:], in1=hi32[:], op=ALU.subtract), [affs, hi32], [lo32])
                op("dve", lambda e: e.tensor_copy(out=affhl[:, :, :, 1], in_=lo32[:]), [lo32], [affhl])
                for i in range(NT):
                    pp = pr[(i // 4) % 2]
                    op("pe", lambda e, i=i, pp=pp: e.transpose(pp[0:16, (i % 4) * 128:(i % 4 + 1) * 128], affs[:, i, :], ident32[:]), [affs, ident32], [pp])
                    if i % 4 == 3:
                        op("dve", lambda e, i=i, pp=pp: e.tensor_copy(out=affT[:, (i - 3) * 128:(i + 1) * 128], in_=pp[0:16, :]), [pp], [affT])
                op("dve", lambda e: e.tensor_copy(out=work[:], in_=affT[:]), [affT], [work])
                for rnd in range(32):
                    op("dve", lambda e: e.max(out=mx8[:], in_=work[:]), [work], [mx8])
                    if rnd < 31:
                        op("dve", lambda e: e.match_replace(out=work[:], in_to_replace=mx8[:], in_values=work[:], imm_value=-1e30), [mx8, work], [work])
                op("dve", lambda e: e.tensor_scalar(out=work[:], in0=affT[:], scalar1=mx8[:, 7:8], scalar2=None, op0=ALU.is_ge), [affT, mx8], [work])
                op("dve", lambda e: e.memset(onesr[:], 1.0), [], [onesr])
                op("dve", lambda e: e.tensor_tensor_scan(out=csb[:], data0=onesr[:], data1=work[:], initial=0.0, op0=ALU.mult, op1=ALU.add), [onesr, work], [csb])
                op("dve", lambda e: e.tensor_tensor(out=csb[:], in0=csb[:], in1=work[:], op=ALU.mult), [csb, work], [csb])
                op("dve", lambda e: e.tensor_copy(out=csm16[:], in_=csb[:]), [csb], [csm16])
                for i in range(NT):
                    pp = pr[(i // 8) % 2]
                    op("pe", lambda e, i=i, pp=pp: e.transpose(pp[:, (i % 8) * 16:(i % 8 + 1) * 16], csb[:, i * 128:(i + 1) * 128], ident32[0:16, 0:16]), [csb, ident32], [pp])
                    if i % 8 == 7:
                        op("dve", lambda e, i=i, pp=pp: e.tensor_copy(out=csmT[:, i - 7:i + 1, :], in_=pp[:, 0:128].rearrange("p (i c) -> p i c", c=16)), [pp], [csmT])
                dump("csm", csb[:], [csb])
                kb.barrier()
            kb.stack = P
            with ExitStack() as P2:
                kb.stack = P2
                Se = kb.sb([128, NT, 256], BF16); xg = kb.sb([128, 8, 256], BF16); hid = kb.sb([128, 16, 256], BF16)
                weg = [kb.sb([128, 8, 256], BF16) for _ in range(2)]; weu = [kb.sb([128, 8, 256], BF16) for _ in range(2)]
                wed = [kb.sb([128, 2, D], BF16) for _ in range(2)]
                sgs = [kb.sb([128, 256]) for _ in range(2)]; gt = kb.sb([128, 4])
                pX = kb.ps([128, 512]); pGa = kb.ps([128, 512]); pUp = kb.ps([128, 512]); pUp2 = kb.ps([128, 512]); pDn = [kb.ps([128, 512]) for _ in range(4)]
                pGas = (pGa, pX); pUps = (pUp, pUp2)

                def do_expert(e_):
                    for i in range(NT):
                        op("dve", lambda e, i=i: e.tensor_scalar(out=Se[:, i, :], in0=iota1[:], scalar1=csmT[:, i, e_:e_ + 1], scalar2=None, op0=ALU.is_equal), [iota1, csmT], [Se])
                    for dk in range(8):
                        for i in range(NT):
                            op("pe", lambda e, i=i, dk=dk: e.matmul(pX[:, 0:256], h2[:, i, dk * 128:(dk + 1) * 128], Se[:, i, :], start=(i == 0), stop=(i == NT - 1)), [h2, Se], [pX])
                        if dk % 2 == 0:
                            op("act", lambda e, dk=dk: e.copy(out=xg[:, dk, :], in_=pX[:, 0:256]), [pX], [xg])
                        else:
                            op("dve", lambda e, dk=dk: e.tensor_copy(out=xg[:, dk, :], in_=pX[:, 0:256]), [pX], [xg])
                    for sc in range(2):
                        for i in range(NT):
                            op("pe", lambda e, i=i, sc=sc: e.matmul(pX[:, 256 + sc * 2:258 + sc * 2], Se[:, i, sc * 128:(sc + 1) * 128], affhl[:, i, e_, :], start=(i == 0), stop=(i == NT - 1)),
                               [Se, affhl], [pX])
                    op("dve", lambda e: e.tensor_copy(out=gt[:], in_=pX[:, 256:260]), [pX], [gt])
                    op("dve", lambda e: e.tensor_tensor(out=gt[:, 0:1], in0=gt[:, 0:1], in1=gt[:, 1:2], op=ALU.add), [gt], [gt])
                    op("dve", lambda e: e.tensor_tensor(out=gt[:, 1:2], in0=gt[:, 2:3], in1=gt[:, 3:4], op=ALU.add), [gt], [gt])
                    def load_w(fb):
                        s_ = (e_ * 8 + fb) % 2
                        fsl = slice(fb * 256, (fb + 1) * 256)
                        dma("pool", weg[s_][:], I["w_eg"][e_].rearrange("(k p) f -> p k f", p=128)[:, :, fsl], [], [weg[s_]], weg[s_])
                        dma("pool", weu[s_][:], I["w_eu"][e_].rearrange("(k p) f -> p k f", p=128)[:, :, fsl], [], [weu[s_]], weu[s_])
                        dma("pool", wed[s_][:], I["w_ed"][e_][fsl, :].rearrange("(c p) d -> p c d", p=128), [], [wed[s_]], wed[s_])

                    def gateup(fch):
                        fb, fc = fch // 2, fch % 2
                        s_ = (e_ * 8 + fb) % 2
                        pg = pGas[fch % 2]; pu = pUps[fch % 2]; sg_ = sgs[fch % 2]
                        for k in range(8):
                            op("pe", lambda e, k=k: e.matmul(pg[:, 0:256], weg[s_][:, k, fc * 128:(fc + 1) * 128], xg[:, k, :], start=(k == 0), stop=(k == 7)), [weg[s_], xg], [pg])
                        for k in range(8):
                            op("pe", lambda e, k=k: e.matmul(pu[:, 0:256], weu[s_][:, k, fc * 128:(fc + 1) * 128], xg[:, k, :], start=(k == 0), stop=(k == 7)), [weu[s_], xg], [pu])
                        op("act", lambda e: e.activation(out=sg_[:], in_=pg[:, 0:256], func=AF.Silu), [pg], [sg_])
                        op("dve", lambda e: e.tensor_tensor(out=hid[:, fch, :], in0=sg_[:], in1=pu[:, 0:256], op=ALU.mult), [sg_, pu], [hid])

                    def down(fch):
                        fb, fc = fch // 2, fch % 2
                        s_ = (e_ * 8 + fb) % 2
                        for sc in range(2):
                            for dh in range(2):
                                op("pe", lambda e, sc=sc, dh=dh: e.matmul(pDn[sc * 2 + dh][:], hid[:, fch, sc * 128:(sc + 1) * 128], wed[s_][:, fc, dh * 512:(dh + 1) * 512],
                                                                         start=(fch == 0), stop=(fch == 15)), [hid, wed[s_]], [pDn[sc * 2 + dh]])

                    for fch in range(16):
                        if fch % 2 == 0:
                            load_w(fch // 2)
                        gateup(fch)
                        if fch >= 1:
                            down(fch - 1)
                    down(15)
                    for sc in range(2):
                        for dh in range(2):
                            op("act", lambda e, sc=sc, dh=dh: e.activation(out=yo[:, e_ * 2 + sc, dh * 512:(dh + 1) * 512], in_=pDn[sc * 2 + dh][:], func=AF.Copy, scale=gt[:, sc:sc + 1]),
                               [pDn[sc * 2 + dh], gt], [yo])

                for e_ in range(16):
                    do_expert(e_)
                kb.barrier()
            kb.stack = P
            with ExitStack() as P3:
                kb.stack = P3
                ST = kb.sb([128, 16, 2, 128], BF16); x1t5 = kb.sb([128, D]); ot = kb.sb([128, D]); junk5 = kb.sb([128, D], BF16); stt5 = kb.sb([128, 8])
                pbc = [kb.ps([128, 512]) for _ in range(4)]; pF = [kb.ps([128, 512]) for _ in range(2)]
                x1v = x1_d.rearrange("(i p) d -> i p d", p=128)
                outv = OUT.rearrange("(i p) d -> i p d", p=128)

                def final_tile(i):
                    isl = slice(i * 128, (i + 1) * 128)
                    dma("sp", x1t5[:], x1v[i], [], [x1t5], x1t5)
                    for e_ in range(16):
                        op("pe", lambda e, e_=e_: e.matmul(pbc[e_ // 4][:, (e_ % 4) * 128:(e_ % 4 + 1) * 128], sel16[:, e_, :], csm16[:, isl], start=True, stop=True), [sel16, csm16], [pbc[e_ // 4]])
                    for b4 in range(4):
                        for sc in range(2):
                            op("dve", lambda e, b4=b4, sc=sc: e.tensor_scalar(out=ST[:, b4 * 4:(b4 + 1) * 4, sc, :], in0=pbc[b4][:].rearrange("p (e c) -> p e c", c=128),
                                                                               scalar1=slotp1[:, sc:sc + 1], scalar2=None, op0=ALU.is_equal), [pbc[b4], slotp1], [ST])
                    for dh in range(2):
                        n = 0
                        for e_ in range(16):
                            for sc in range(2):
                                op("pe", lambda e, e_=e_, sc=sc, dh=dh, n=n: e.matmul(pF[dh][:], ST[:, e_, sc, :], yo[:, e_ * 2 + sc, dh * 512:(dh + 1) * 512], start=(n == 0), stop=(n == 31)),
                                   [ST, yo], [pF[dh]])
                                n += 1
                    op("act", lambda e: e.activation(out=junk5[:, 0:512], in_=pF[0][:], func=AF.Square, accum_out=stt5[:, 0:1]), [pF[0]], [junk5, stt5])
                    op("act", lambda e: e.activation(out=junk5[:, 512:1024], in_=pF[1][:], func=AF.Square, accum_out=stt5[:, 4:5]), [pF[1]], [junk5, stt5])
                    op("dve", lambda e: e.tensor_tensor(out=stt5[:, 0:1], in0=stt5[:, 0:1], in1=stt5[:, 4:5], op=ALU.add), [stt5], [stt5])
                    op("dve", lambda e: e.tensor_scalar(out=stt5[:, 1:2], in0=stt5[:, 0:1], scalar1=1.0 / D, scalar2=EPS, op0=ALU.mult, op1=ALU.add), [stt5], [stt5])
                    op("act", lambda e: e.sqrt(out=stt5[:, 2:3], in_=stt5[:, 1:2]), [stt5], [stt5])
                    op("dve", lambda e: e.reciprocal(out=stt5[:, 3:4], in_=stt5[:, 2:3]), [stt5], [stt5])
                    for dh in range(2):
                        hs = slice(dh * 512, (dh + 1) * 512)
                        op("dve", lambda e, dh=dh, hs=hs: e.scalar_tensor_tensor(out=ot[:, hs], in0=pF[dh][:], scalar=stt5[:, 3:4], in1=modbc[:, 3, hs], op0=ALU.mult, op1=ALU.mult),
                           [pF[dh], stt5, modbc], [ot])
                    op("dve", lambda e: e.tensor_tensor(out=ot[:], in0=ot[:], in1=x1t5[:], op=ALU.add), [ot, x1t5], [ot])
                    dma("sp", outv[i], ot[:], [ot], [], ot)

                for i in range(NT):
                    final_tile(i)
                kb.barrier()
            kb.stack = P
          kb.stack = G

        if stop_after >= 99:
            pass
        kb.barrier()
        kb.emit()
    return nc


def kernel(**inputs):
    inp = {k: np.asarray(v, dtype=np.float32) for k, v in inputs.items()}
    shared = _host_prep(inp)
    import os
    nc = build_program(stop_after=int(os.environ.get("MK_STOP", "99")))
    nb = inp["x"].shape[0]
    in_maps = []
    for b in range(nb):
        m = dict(shared)
        m["x"] = np.ascontiguousarray(inp["x"][b])
        m["ctx"] = np.ascontiguousarray(inp["ctx"][b])
        cc = np.stack([inp["c"][b].reshape(8, 128).T, inp["c_ctx"].reshape(8, 128).T], axis=2).reshape(128, 16)
        m["cc"] = np.ascontiguousarray(cc)
        in_maps.append(m)
    res = run_bass_kernel_spmd(nc, in_maps, core_ids=list(range(nb)))
    out = np.stack([np.asarray(res.results[b]["out"]) for b in range(nb)], axis=0)
    return out.astype(np.float32)
```

```python
from contextlib import ExitStack
import numpy as np
import concourse.bass as bass
import concourse.mybir as mybir
from concourse.bass_utils import run_bass_kernel_spmd

F32 = mybir.dt.float32
BF16 = mybir.dt.bfloat16
AF = mybir.ActivationFunctionType
ALU = mybir.AluOpType
AX = mybir.AxisListType

D = 1024
T = 2048
NT = 16
CT = 256
EPS = 1e-6
NCOL = 10304
DBG = {}
STOP_AFTER = [99]


class Dep:
    __slots__ = ("w", "r")

    def __init__(self):
        self.w = None
        self.r = {}


class TT:
    def __init__(self, t):
        self.t = t
        self.d = Dep()

    def __getitem__(self, k):
        return self.t[k]


class KB:
    ENG = ("pe", "act", "dve", "pool", "sp")

    def __init__(self, nc, gstack):
        self.nc = nc
        self.g = gstack
        self.stack = gstack
        self.sems = {}
        self.cnt = {}
        for e in self.ENG:
            self.sems[e] = gstack.enter_context(nc.semaphore("s_" + e))
            self.cnt[e] = 0
        self.prog = {e: [] for e in self.ENG}
        self.seen = {e: {} for e in self.ENG}
        self.n = 0

    def sb(self, shape, dtype=F32):
        self.n += 1
        return TT(self.stack.enter_context(self.nc.sbuf_tensor(f"sb{self.n}", list(shape), dtype)))

    def ps(self, shape=(128, 512), dtype=F32):
        self.n += 1
        return TT(self.stack.enter_context(self.nc.psum_tensor(f"ps{self.n}", list(shape), dtype)))

    def _deps(self, lst):
        return [x.d if isinstance(x, TT) else x for x in lst]

    def _waits(self, eng, reads, writes):
        need = {}

        def add(k, v):
            if k == "pe" and eng == "pe":
                return
            if need.get(k, 0) < v:
                need[k] = v

        for d in reads:
            if d.w is not None:
                add(*d.w)
        for d in writes:
            if d.w is not None:
                add(*d.w)
            for k, v in d.r.items():
                add(k, v)
        out = []
        sn = self.seen[eng]
        for k, v in need.items():
            if sn.get(k, 0) < v:
                sn[k] = v
                out.append((k, v))
        return out

    def _mark(self, tok, reads, writes):
        k, v = tok
        for d in reads:
            if d.r.get(k, 0) < v:
                d.r[k] = v
        for d in writes:
            d.w = tok
            d.r = {}

    def op(self, eng, fn, R=(), W=()):
        R = self._deps(R)
        W = self._deps(W)
        waits = self._waits(eng, R, W)
        self.cnt[eng] += 1
        self.prog[eng].append((waits, fn, eng, 1))
        self._mark((eng, self.cnt[eng]), R, W)

    def dma(self, q, out, in_, R, W, semo):
        R = self._deps(R)
        W = self._deps(W)
        waits = self._waits(q, R, W)
        key = ("d", id(semo.d if isinstance(semo, TT) else semo))
        if key not in self.sems:
            self.sems[key] = self.g.enter_context(self.nc.semaphore(f"d{len(self.sems)}"))
            self.cnt[key] = 0
        self.cnt[key] += 16
        self.prog[q].append((waits, lambda e: e.dma_start(out=out, in_=in_), key, 16))
        self._mark((key, self.cnt[key]), R, W)

    def barrier(self):
        allk = [(k, v) for k, v in self.cnt.items() if v > 0]
        for e in self.ENG:
            waits = []
            for k, v in allk:
                if k == e:
                    continue
                if self.seen[e].get(k, 0) < v:
                    self.seen[e][k] = v
                    waits.append((k, v))
            self.prog[e].append((waits, None, None, 0))

    def emit(self):
        def replay(e, name):
            for waits, fn, sk, inc in self.prog[name]:
                for k, v in waits:
                    e.wait_ge(self.sems[k], v)
                if fn is not None:
                    fn(e).then_inc(self.sems[sk], inc)

        with self.nc.Block() as block:
            @block.tensor
            def _(e):
                replay(e, "pe")

            @block.scalar
            def _(e):
                replay(e, "act")

            @block.vector
            def _(e):
                replay(e, "dve")

            @block.gpsimd
            def _(e):
                replay(e, "pool")

            @block.sync
            def _(e):
                replay(e, "sp")


def _rope_tables():
    p = np.arange(128)
    dim = p % 64
    axis = dim // 32
    i = dim % 32
    j = i % 16
    inv = (10000.0 ** (-(j.astype(np.float32)) / np.float32(16))).astype(np.float32)
    t = np.arange(T)
    rows = (t // 64).astype(np.float32)
    cols = (t % 64).astype(np.float32)
    pos = np.where(axis[:, None] == 0, rows[None, :], cols[None, :]).astype(np.float32)
    ang = (pos * inv[:, None]).astype(np.float32)
    cos = np.cos(ang).astype(np.float32)
    sin = np.sin(ang).astype(np.float32)
    sgn = np.where(i < 16, -1.0, 1.0).astype(np.float32)
    return cos, (sin * sgn[:, None]).astype(np.float32)


def _perm64():
    d = np.arange(64)
    i = d % 32
    return np.where(i < 16, d + 16, d - 16)


def _host_prep(inp):
    f = np.float32
    sh = {}
    sh["w_ada"] = np.ascontiguousarray(inp["w_ada"][0])
    sh["b_adaT"] = np.ascontiguousarray(inp["b_ada"][0].reshape(48, 128).T)
    sh["b_ada_bc"] = np.ascontiguousarray(np.broadcast_to(inp["b_ada"][0][2048:6144][None, :], (128, 4096)))
    sh["npmT"] = np.ascontiguousarray(inp["norm_pre_mix"][0].reshape(8, 128).T)
    vec = np.stack([inp["norm_post_mix"][0], inp["norm_pre_ffn"][0], inp["norm_post_ffn"][0]])
    sh["vec_bc"] = np.ascontiguousarray(np.broadcast_to(vec.reshape(1, 3 * 1024), (128, 3 * 1024)))
    w_in = inp["w_in"][0]
    sh["w_in"] = np.ascontiguousarray(w_in)
    perm = _perm64()
    colperm = np.concatenate([h * 64 + perm for h in range(16)])
    sh["w_qkp"] = np.ascontiguousarray(np.concatenate([w_in[:, 0:1024][:, colperm], w_in[:, 1024:2048][:, colperm]], axis=1))
    cos, sin = _rope_tables()
    sh["cosT"] = cos
    sh["sinT"] = sin
    rpb = inp["na_rpb"][0]
    kc = np.arange(64)[:, None]
    qc = np.arange(64)[None, :]
    cidx = np.clip(kc - qc + 15, 0, 30)
    rb = np.zeros((128, 16, 14, 64), f)
    for kp in range(2):
        for dd in range(14):
            rb[kp * 64:(kp + 1) * 64, :, dd, :] = np.transpose(rpb[:, dd + kp, :][:, cidx], (1, 0, 2))
    sh["rb"] = np.ascontiguousarray(rb.reshape(128, 16, 7, 2, 64).transpose(0, 1, 3, 2, 4).reshape(128, 16 * 14 * 64))
    cstart = np.clip(np.arange(64) - 8, 0, 48)[None, :]
    m = ((kc >= cstart) & (kc < cstart + 16)).astype(f)
    sh["mask01"] = np.ascontiguousarray(np.concatenate([m, m], axis=0))
    cw = inp["ssd_conv_w"][0]
    sh["convwT"] = np.ascontiguousarray(np.transpose(cw.reshape(5, 24, 128), (2, 1, 0)).reshape(128, 120))
    sh["convbT"] = np.ascontiguousarray(inp["ssd_conv_b"][0].reshape(24, 128).T)
    dtb = np.concatenate([inp["ssd_dt_bias_fwd"][0], inp["ssd_dt_bias_bwd"][0]])
    alog = np.concatenate([inp["ssd_a_log_fwd"][0], inp["ssd_a_log_bwd"][0]])
    sh["dtb_bc"] = np.ascontiguousarray(np.broadcast_to(dtb[None, :], (128, 64)))
    sh["alog_bc"] = np.ascontiguousarray(np.broadcast_to(alog[None, :], (128, 64)))
    sh["dsk_bc"] = np.ascontiguousarray(np.broadcast_to(inp["ssd_d_skip"][0][None, :], (128, 32)))
    sh["ssdn_bc"] = np.ascontiguousarray(np.broadcast_to(inp["ssd_norm"][0][None, :], (128, 2048)))
    sh["w_bna"] = np.ascontiguousarray(inp["w_branch_na"][0])
    sh["w_bssd"] = np.ascontiguousarray(inp["w_branch_ssd"][0])
    sh["w_out"] = np.ascontiguousarray(inp["w_out"][0])
    sh["w_router"] = np.ascontiguousarray(inp["w_router"][0])
    sh["w_eg"] = np.ascontiguousarray(inp["w_exp_gate"][0])
    sh["w_eu"] = np.ascontiguousarray(inp["w_exp_up"][0])
    sh["w_ed"] = np.ascontiguousarray(inp["w_exp_down"][0])
    sh["ident"] = np.eye(128, dtype=f)
    l = np.arange(128)
    sh["triu"] = (l[:, None] <= l[None, :]).astype(f)
    sh["tril"] = (l[:, None] >= l[None, :]).astype(f)
    sh["iota1"] = np.ascontiguousarray(np.broadcast_to(np.arange(1, 257, dtype=f)[None, :], (128, 256)))
    sh["slotp1"] = np.stack([l + 1, l + 129], axis=1).astype(f)
    sel = np.zeros((16, 16, 128), f)
    for e in range(16):
        sel[e, e, :] = 1.0
    sh["sel16"] = np.ascontiguousarray(sel.reshape(16, 2048))
    return sh


SHAPES = {
    "x": [T, D], "ctx": [CT, D], "cc": [128, 16],
    "w_ada": [D, 6144], "b_adaT": [128, 48], "b_ada_bc": [128, 4096], "npmT": [128, 8], "vec_bc": [128, 3072],
    "w_in": [D, NCOL], "w_qkp": [D, 2048], "cosT": [128, T], "sinT": [128, T], "rb": [128, 16 * 14 * 64],
    "mask01": [128, 64], "convwT": [128, 120], "convbT": [128, 24], "dtb_bc": [128, 64], "alog_bc": [128, 64],
    "dsk_bc": [128, 32], "ssdn_bc": [128, 2048], "w_bna": [D, D], "w_bssd": [2048, D], "w_out": [D, D],
    "w_router": [D, 16], "w_eg": [16, D, 2048], "w_eu": [16, D, 2048], "w_ed": [16, 2048, D],
    "ident": [128, 128], "triu": [128, 128], "tril": [128, 128], "iota1": [128, 256], "slotp1": [128, 2],
    "sel16": [16, 2048],
}


def build_program(dbg=None, stop_after=99, only=None, skip=()):
    dbg = dbg or {}
    nc = bass.Bass("TRN2", target_bir_lowering=False)
    I = {k: nc.dram_tensor(k, v, F32, kind="ExternalInput").ap() for k, v in SHAPES.items() if only is None or k in only}
    OUT = nc.dram_tensor("out", [T, D], F32, kind="ExternalOutput").ap()
    DO = {k: nc.dram_tensor("dbg_" + k, list(v[0]), v[1], kind="ExternalOutput").ap() for k, v in dbg.items()}
    onaT_d = nc.dram_tensor("onaT_d", [8, 128, T], BF16).ap()
    ynT_d = nc.dram_tensor("ynT_d", [16, 128, T], BF16).ap()
    x1_d = nc.dram_tensor("x1_d", [T, D], F32).ap()
    h2_d = nc.dram_tensor("h2_d", [T, D], BF16).ap()

    with ExitStack() as G:
        kb = KB(nc, G)
        op = kb.op
        dma = kb.dma
        dbgsem = TT(None)

        def dump(name, src_ap, R):
            if name in DO:
                dma("sp", DO[name], src_ap, R, [], dbgsem)

        ident16 = kb.sb([128, 128], BF16)
        ident32 = kb.sb([128, 128])
        ones16 = kb.sb([128, 128], BF16)
        ones32 = kb.sb([128, 128])
        hT = kb.sb([128, 8, T], BF16)
        hcT = kb.sb([128, 8, CT], BF16)
        dma("sp", ident32[:], I["ident"], [], [ident32], ident32)
        dma("pool", ident16[:], I["ident"], [], [ident16], ident16)
        op("dve", lambda e: e.memset(ones32[:], 1.0), [], [ones32])
        op("dve", lambda e: e.memset(ones16[:], 1.0), [], [ones16])

        with ExitStack() as P:
            kb.stack = P
            cc = kb.sb([128, 16])
            sc16 = kb.sb([128, 16], BF16)
            badT = kb.sb([128, 48])
            npmT = kb.sb([128, 8])
            dma("sp", cc[:], I["cc"], [], [cc], cc)
            dma("sp", badT[:], I["b_adaT"], [], [badT], badT)
            dma("sp", npmT[:], I["npmT"], [], [npmT], npmT)
            op("act", lambda e: e.activation(out=sc16[:], in_=cc[:], func=AF.Silu), [cc], [sc16])
            wa = [kb.sb([128, 8, 1024], BF16) for _ in range(2)]
            wav = I["w_ada"].rearrange("(k p) n -> p k n", p=128)
            for g in range(2):
                dma("pool", wa[g][:], wav[:, :, g * 1024:(g + 1) * 1024], [], [wa[g]], wa[g])
            mps = kb.ps([128, 512])
            for g in range(2):
                for jj in range(8):
                    j = g * 8 + jj
                    for k in range(8):
                        op("pe", lambda e, g=g, jj=jj, j=j, k=k: e.matmul(
                            mps[:, 2 * j:2 * j + 2], wa[g][:, k, jj * 128:(jj + 1) * 128], sc16[:, 2 * k:2 * k + 2],
                            start=(k == 0), stop=(k == 7)), [wa[g], sc16], [mps])
            modT = kb.sb([128, 16, 2])
            op("dve", lambda e: e.tensor_tensor(
                out=modT[:], in0=mps[:, 0:32].rearrange("p (j w) -> p j w", w=2),
                in1=badT[:, 0:16].unsqueeze(2).to_broadcast([128, 16, 2]), op=ALU.add), [mps, badT], [modT])
            a1T = kb.sb([128, 8, 2])
            op("dve", lambda e: e.tensor_scalar(out=a1T[:], in0=modT[:, 8:16, :], scalar1=1.0, scalar2=None, op0=ALU.add),
               [modT], [a1T])
            op("dve", lambda e: e.tensor_tensor(out=a1T[:], in0=a1T[:], in1=npmT[:].unsqueeze(2).to_broadcast([128, 8, 2]),
                                                op=ALU.mult), [a1T, npmT], [a1T])
            xt = [kb.sb([128, D]) for _ in range(2)]
            junk = kb.sb([128, D], BF16)
            xs16 = [kb.sb([128, D], BF16) for _ in range(2)]
            st = [kb.sb([128, 4]) for _ in range(2)]
            tps = [kb.ps([128, 1024], BF16) for _ in range(2)]
            xv = I["x"].rearrange("(i p) d -> i p d", p=128)
            cv = I["ctx"].rearrange("(i p) d -> i p d", p=128)
            for i in range(NT + 2):
                s = i % 2
                isx = i < NT
                src = xv[i] if isx else cv[i - NT]
                w = 0 if isx else 1
                dst = hT if isx else hcT
                c0 = (i if isx else i - NT) * 128
                dma("sp", xt[s][:], src, [], [xt[s]], xt[s])
                op("act", lambda e, s=s: e.activation(out=junk[:], in_=xt[s][:], func=AF.Square, accum_out=st[s][:, 0:1]),
                   [xt[s]], [junk, st[s]])
                op("dve", lambda e, s=s: e.tensor_scalar(out=st[s][:, 1:2], in0=st[s][:, 0:1], scalar1=1.0 / D, scalar2=EPS,
                                                         op0=ALU.mult, op1=ALU.add), [st[s]], [st[s]])
                op("act", lambda e, s=s: e.sqrt(out=st[s][:, 2:3], in_=st[s][:, 1:2]), [st[s]], [st[s]])
                op("dve", lambda e, s=s: e.reciprocal(out=st[s][:, 3:4], in_=st[s][:, 2:3]), [st[s]], [st[s]])
                op("act", lambda e, s=s: e.activation(out=xs16[s][:], in_=xt[s][:], func=AF.Copy, scale=st[s][:, 3:4]),
                   [xt[s], st[s]], [xs16[s]])
                for k in range(8):
                    op("pe", lambda e, s=s, k=k: e.transpose(tps[s][:, k * 128:(k + 1) * 128], xs16[s][:, k * 128:(k + 1) * 128],
                                                             ident16[:]), [xs16[s], ident16], [tps[s]])
                for k in range(8):
                    eng = "dve" if k % 2 == 0 else "act"
                    if eng == "dve":
                        op("dve", lambda e, s=s, k=k, dst=dst, c0=c0, w=w: e.tensor_scalar(
                            out=dst[:, k, c0:c0 + 128], in0=tps[s][:, k * 128:(k + 1) * 128],
                            scalar1=a1T[:, k, w:w + 1], scalar2=modT[:, k, w:w + 1], op0=ALU.mult, op1=ALU.add),
                           [tps[s], a1T, modT], [dst])
                    else:
                        op("act", lambda e, s=s, k=k, dst=dst, c0=c0, w=w: e.activation(
                            out=dst[:, k, c0:c0 + 128], in_=tps[s][:, k * 128:(k + 1) * 128], func=AF.Identity,
                            bias=modT[:, k, w:w + 1], scale=a1T[:, k, w:w + 1]), [tps[s], a1T, modT], [dst])
            kb.barrier()
        kb.stack = G
        if "hT" in DO:
            dump("hT", hT[:], [hT])
            dump("hcT", hcT[:], [hcT])


        if stop_after >= 2 and 2 not in skip:
          with ExitStack() as P:
            kb.stack = P
            winv = I["w_in"].rearrange("(k p) n -> p k n", p=128)
            triu = kb.sb([128, 128]); tril = kb.sb([128, 128])
            dma("sp", triu[:], I["triu"], [], [triu], triu)
            dma("sp", tril[:], I["tril"], [], [tril], tril)
            convw = kb.sb([128, 24, 5]); convb = kb.sb([128, 24])
            dma("sp", convw[:], I["convwT"].rearrange("p (c j) -> p c j", j=5), [], [convw], convw)
            dma("sp", convb[:], I["convbT"], [], [convb], convb)
            dtb = kb.sb([128, 64]); Aneg = kb.sb([128, 64]); dsk = kb.sb([128, 32]); ssdn = kb.sb([128, 512])
            dma("sp", dtb[:], I["dtb_bc"], [], [dtb], dtb)
            dma("sp", Aneg[:], I["alog_bc"], [], [Aneg], Aneg)
            dma("sp", dsk[:], I["dsk_bc"], [], [dsk], dsk)
            op("act", lambda e: e.activation(out=Aneg[:], in_=Aneg[:], func=AF.Exp), [Aneg], [Aneg])
            op("dve", lambda e: e.tensor_scalar(out=Aneg[:], in0=Aneg[:], scalar1=-1.0, scalar2=None, op0=ALU.mult), [Aneg], [Aneg])
            NTT = NT + 2
            wdt = kb.sb([128, 8, 64], BF16)
            dma("pool", wdt[:], winv[:, :, 8192:8256], [], [wdt], wdt)
            dtA = kb.sb([128, NTT, 64]); aA = kb.sb([128, NTT, 64])
            pin = [kb.ps([128, 512]) for _ in range(2)]
            pdt = pin[0]
            for i in range(NTT):
                src = hT if i < NT else hcT
                c0 = (i if i < NT else i - NT) * 128
                for k in range(8):
                    op("pe", lambda e, i=i, k=k, src=src, c0=c0: e.matmul(pdt[:, (i % 8) * 64:(i % 8) * 64 + 64], src[:, k, c0:c0 + 128],
                                                                         wdt[:, k, :], start=(k == 0), stop=(k == 7)), [src, wdt], [pdt])
                if i % 8 == 7 or i == NTT - 1:
                    i0 = (i // 8) * 8
                    n = i - i0 + 1
                    op("dve", lambda e, i0=i0, n=n: e.tensor_tensor(
                        out=dtA[:, i0:i0 + n, :], in0=pdt[:, 0:n * 64].rearrange("p (i c) -> p i c", c=64),
                        in1=dtb[:].unsqueeze(1).to_broadcast([128, n, 64]), op=ALU.add), [pdt, dtb], [dtA])
            op("act", lambda e: e.activation(out=dtA[:], in_=dtA[:], func=AF.Exp), [dtA], [dtA])
            op("act", lambda e: e.activation(out=dtA[:], in_=dtA[:], func=AF.Ln, bias=1.0), [dtA], [dtA])
            op("dve", lambda e: e.tensor_tensor(out=aA[:], in0=dtA[:], in1=Aneg[:].unsqueeze(1).to_broadcast([128, NTT, 64]),
                                                op=ALU.mult), [dtA, Aneg], [aA])
            dump("dt", dtA[:], [dtA])

            wz = kb.sb([128, 8, 512], BF16); wx = kb.sb([128, 8, 512], BF16)
            wB = kb.sb([128, 8, 128], BF16); wC = kb.sb([128, 8, 128], BF16)
            stages = [kb.sb([128, T + 4], BF16) for _ in range(2)]; acc = kb.sb([128, T]); xsTc = kb.sb([128, T], BF16)
            stage_ctr = [0]
            stage_c = kb.sb([128, CT + 4]); acc_c = kb.sb([128, CT]); xsTc_c = kb.sb([128, CT], BF16)
            BT = kb.sb([128, T], BF16); CTt = kb.sb([128, T], BF16)
            BT_c = kb.sb([128, CT], BF16); CT_c = kb.sb([128, CT], BF16)
            xtok = kb.sb([128, NTT, 512], BF16); Btok = kb.sb([128, NTT, 128], BF16)
            yf = kb.sb([128, NT, 512], BF16); ynT = kb.sb([128, 4, T], BF16)
            acs = kb.sb([128, NTT, 16]); tot = kb.sb([128, NTT, 16]); eacs = kb.sb([128, NTT, 16])
            dte = kb.sb([128, NTT, 16]); etot = kb.sb([128, NTT, 16]); ebias = kb.sb([128, NTT, 16])
            ag = kb.sb([128, NTT, 16]); lng = kb.sb([128, NTT, 16]); dtg = kb.sb([128, NTT, 16])
            Dm = kb.sb([128, 8, 128]); t2 = kb.sb([128, 8, 128]); MT = kb.sb([128, 8, 128], BF16)
            cbm = [kb.sb([128, 128]) for _ in range(2)]
            tmp1 = kb.sb([128, 512]); ysum = kb.sb([128, 512]); xd = kb.sb([128, 512], BF16); dskx = kb.sb([128, 512])
            hst = [kb.sb([128, 512]) for _ in range(2)]; hbf = [kb.sb([128, 512], BF16) for _ in range(2)]
            ub = kb.sb([128, 512]); zs = kb.sb([128, 512]); yn16 = kb.sb([128, 512], BF16); gst = kb.sb([128, 4])
            zs4 = kb.sb([128, 4, 512], BF16); gst4 = kb.sb([128, 16])
            for stg_ in stages:
                op("dve", lambda e, stg_=stg_: e.memset(stg_[:], 0.0), [], [stg_])
            op("dve", lambda e: e.memset(stage_c[:], 0.0), [], [stage_c])
            ptr32 = kb.ps([128, 512])
            ptr = TT(ptr32[:].bitcast(BF16)); ptr.d = ptr32.d
            pR = kb.ps([128, 1024])
            pY = kb.ps([128, 512]); pO = kb.ps([128, 512]); pS = kb.ps([128, 512])

            def inproj_fm(wt, wcols, dstps, src, c0, n):
                for k in range(8):
                    op("pe", lambda e, k=k: e.matmul(dstps[:, 0:n], wt[:, k, wcols], src[:, k, c0:c0 + n],
                                                     start=(k == 0), stop=(k == 7)), [wt, src], [dstps])

            def conv_chunk(wt, wcols, cc_idx, dstT, dstT_c):
                stage = stages[stage_ctr[0] % 2]
                stage_ctr[0] += 1
                for tb in range(4):
                    pp = pin[tb % 2]
                    inproj_fm(wt, wcols, pp, hT, tb * 512, 512)
                    op("act", lambda e, pp=pp, tb=tb: e.copy(out=stage[:, 2 + tb * 512:2 + (tb + 1) * 512], in_=pp[:]), [pp], [stage])
                pp = pin[0]
                inproj_fm(wt, wcols, pp, hcT, 0, CT)
                op("act", lambda e, pp=pp: e.copy(out=stage_c[:, 2:2 + CT], in_=pp[:, 0:CT]), [pp], [stage_c])
                for (stg, ac, n, dst) in ((stage, acc, T, dstT), (stage_c, acc_c, CT, dstT_c)):
                    op("act", lambda e, stg=stg, ac=ac, n=n: e.activation(out=ac[:], in_=stg[:, 0:n], func=AF.Copy, scale=convw[:, cc_idx, 0:1]),
                       [stg, convw], [ac])
                    for j in range(1, 5):
                        op("dve", lambda e, stg=stg, ac=ac, n=n, j=j: e.scalar_tensor_tensor(
                            out=ac[:], in0=stg[:, j:j + n], scalar=convw[:, cc_idx, j:j + 1], in1=ac[:], op0=ALU.mult, op1=ALU.add),
                           [stg, convw, ac], [ac])
                    op("act", lambda e, ac=ac, dst=dst: e.activation(out=dst[:], in_=ac[:], func=AF.Silu, bias=convb[:, cc_idx:cc_idx + 1]),
                       [ac, convb], [dst])

            def do_group(g):
                dma("pool", wz[:], winv[:, :, 3072 + g * 512:3072 + (g + 1) * 512], [], [wz], wz)
                dma("pool", wx[:], winv[:, :, 5120 + g * 512:5120 + (g + 1) * 512], [], [wx], wx)
                dma("pool", wB[:], winv[:, :, 7168 + g * 128:7168 + (g + 1) * 128], [], [wB], wB)
                dma("pool", wC[:], winv[:, :, 7680 + g * 128:7680 + (g + 1) * 128], [], [wC], wC)
                dma("sp", ssdn[:], I["ssdn_bc"][:, g * 512:(g + 1) * 512], [], [ssdn], ssdn)
                for j in range(4):
                    conv_chunk(wx, slice(j * 128, (j + 1) * 128), g * 4 + j, xsTc, xsTc_c)
                    for i0 in range(0, NTT, 8):
                        n = min(8, NTT - i0)
                        for ii in range(n):
                            i = i0 + ii
                            srcT = xsTc if i < NT else xsTc_c
                            c0 = (i if i < NT else i - NT) * 128
                            op("pe", lambda e, ii=ii, srcT=srcT, c0=c0: e.transpose(ptr[:, ii * 128:(ii + 1) * 128], srcT[:, c0:c0 + 128], ident16[:]),
                               [srcT, ident16], [ptr])
                        op("dve", lambda e, i0=i0, n=n, j=j: e.tensor_copy(out=xtok[:, i0:i0 + n, j * 128:(j + 1) * 128],
                                                                         in_=ptr[:, 0:n * 128].rearrange("p (i c) -> p i c", c=128)), [ptr], [xtok])
                conv_chunk(wB, slice(0, 128), 16 + g, BT, BT_c)
                conv_chunk(wC, slice(0, 128), 20 + g, CTt, CT_c)
                for i0 in range(0, NTT, 8):
                    n = min(8, NTT - i0)
                    for ii in range(n):
                        i = i0 + ii
                        srcT = BT if i < NT else BT_c
                        c0 = (i if i < NT else i - NT) * 128
                        op("pe", lambda e, ii=ii, srcT=srcT, c0=c0: e.transpose(ptr[:, ii * 128:(ii + 1) * 128], srcT[:, c0:c0 + 128], ident16[:]),
                           [srcT, ident16], [ptr])
                    op("dve", lambda e, i0=i0, n=n: e.tensor_copy(out=Btok[:, i0:i0 + n, :],
                                                                 in_=ptr[:, 0:n * 128].rearrange("p (i c) -> p i c", c=128)), [ptr], [Btok])
                if g == 0:
                    dump("xtok", xtok[:], [xtok]); dump("Btok", Btok[:], [Btok]); dump("CT", CTt[:], [CTt])
                for (dst, srcA) in ((ag, aA), (dtg, dtA)):
                    op("dve", lambda e, dst=dst, srcA=srcA: e.tensor_copy(out=dst[:, :, 0:8], in_=srcA[:, :, g * 8:g * 8 + 8]), [srcA], [dst])
                    op("dve", lambda e, dst=dst, srcA=srcA: e.tensor_copy(out=dst[:, :, 8:16], in_=srcA[:, :, 32 + g * 8:32 + g * 8 + 8]), [srcA], [dst])
                op("act", lambda e: e.activation(out=lng[:], in_=dtg[:], func=AF.Ln), [dtg], [lng])
                pc = pin[0]; pt = pin[1]
                for i0 in range(0, NTT, 9):
                    for ii in range(9):
                        i = i0 + ii
                        op("pe", lambda e, i=i, ii=ii: e.matmul(pc[:, ii * 16:ii * 16 + 8], triu[:], ag[:, i, 0:8], start=True, stop=True), [triu, ag], [pc])
                        op("pe", lambda e, i=i, ii=ii: e.matmul(pc[:, ii * 16 + 8:ii * 16 + 16], tril[:], ag[:, i, 8:16], start=True, stop=True), [tril, ag], [pc])
                        op("pe", lambda e, i=i, ii=ii: e.matmul(pt[:, ii * 16:ii * 16 + 16], ones32[:], ag[:, i, :], start=True, stop=True), [ones32, ag], [pt])
                    op("dve", lambda e, i0=i0: e.tensor_copy(out=acs[:, i0:i0 + 9, :], in_=pc[:, 0:144].rearrange("p (i c) -> p i c", c=16)), [pc], [acs])
                    op("dve", lambda e, i0=i0: e.tensor_copy(out=tot[:, i0:i0 + 9, :], in_=pt[:, 0:144].rearrange("p (i c) -> p i c", c=16)), [pt], [tot])
                op("act", lambda e: e.activation(out=eacs[:], in_=acs[:], func=AF.Exp), [acs], [eacs])
                op("act", lambda e: e.activation(out=etot[:], in_=tot[:], func=AF.Exp), [tot], [etot])
                op("dve", lambda e: e.tensor_tensor(out=dte[:], in0=tot[:], in1=acs[:], op=ALU.subtract), [tot, acs], [dte])
                op("act", lambda e: e.activation(out=dte[:], in_=dte[:], func=AF.Exp), [dte], [dte])
                op("dve", lambda e: e.tensor_tensor(out=dte[:], in0=dte[:], in1=dtg[:], op=ALU.mult), [dte, dtg], [dte])
                op("dve", lambda e: e.tensor_tensor(out=ebias[:], in0=lng[:], in1=acs[:], op=ALU.subtract), [lng, acs], [ebias])

                def state_update(tile, dr):
                    cs = slice(dr * 8, dr * 8 + 8)
                    op("pool", lambda e: e.tensor_tensor(out=xd[:].rearrange("p (r c) -> p r c", c=64),
                                                        in0=xtok[:, tile, :].rearrange("p (r c) -> p r c", c=64),
                                                        in1=dte[:, tile, cs].unsqueeze(2).to_broadcast([128, 8, 64]), op=ALU.mult), [xtok, dte], [xd])
                    op("pe", lambda e: e.matmul(pO[:], Btok[:, tile, :], xd[:], start=True, stop=True), [Btok, xd], [pO])
                    op("dve", lambda e: e.tensor_tensor(out=hst[dr][:].rearrange("p (r c) -> p r c", c=64),
                                                        in0=hst[dr][:].rearrange("p (r c) -> p r c", c=64),
                                                        in1=etot[:, tile, cs].unsqueeze(2).to_broadcast([128, 8, 64]), op=ALU.mult), [hst[dr], etot], [hst[dr]])
                    op("dve", lambda e: e.tensor_tensor(out=hst[dr][:], in0=hst[dr][:], in1=pO[:], op=ALU.add), [hst[dr], pO], [hst[dr]])
                    op("act", lambda e: e.copy(out=hbf[dr][:], in_=hst[dr][:]), [hst[dr]], [hbf[dr]])

                for dr in range(2):
                    op("dve", lambda e, dr=dr: e.memset(hst[dr][:], 0.0), [], [hst[dr]])
                    op("dve", lambda e, dr=dr: e.memset(hbf[dr][:], 0.0), [], [hbf[dr]])
                state_update(NT, 0); state_update(NT + 1, 0)
                state_update(NT + 1, 1); state_update(NT, 1)
                if g == 0:
                    dump("hf", hst[0][:], [hst[0]]); dump("hb", hst[1][:], [hst[1]])

                tris = (triu, tril)
                pYs = (pY, pS)

                def prepA(dr, c):
                    cs0 = dr * 8
                    csl = slice(c * 128, (c + 1) * 128)
                    pcb = pin[0]
                    cb = cbm[0]
                    op("pe", lambda e: e.matmul(pcb[:, 0:128], BT[:, csl], CTt[:, csl], start=True, stop=True), [BT, CTt], [pcb])
                    op("dve", lambda e: e.tensor_tensor(out=cb[:], in0=pcb[:, 0:128], in1=tris[dr][:], op=ALU.mult), [pcb, tris[dr]], [cb])
                    op("pool", lambda e: e.tensor_tensor(out=Dm[:], in0=ident32[:].unsqueeze(1).to_broadcast([128, 8, 128]),
                                                        in1=acs[:, c, cs0:cs0 + 8].unsqueeze(2).to_broadcast([128, 8, 128]), op=ALU.mult),
                       [ident32, acs], [Dm])
                    for hh in range(2):
                        op("pe", lambda e, hh=hh: e.matmul(pR[:, hh * 512:(hh + 1) * 512], ones32[:],
                                                           Dm[:, hh * 4:(hh + 1) * 4, :].rearrange("p r l -> p (r l)"), start=True, stop=True),
                           [ones32, Dm], [pR])

                def prepB(dr, c):
                    cs0 = dr * 8
                    cb = cbm[0]
                    py = pYs[dr]
                    for r in range(8):
                        op("act", lambda e, r=r: e.activation(out=t2[:, r, :], in_=pR[:, r * 128:(r + 1) * 128], func=AF.Exp,
                                                              bias=ebias[:, c, cs0 + r:cs0 + r + 1]), [pR, ebias], [t2])
                    op("dve", lambda e: e.scalar_tensor_tensor(out=MT[:], in0=t2[:], scalar=1e30,
                                                               in1=cb[:].unsqueeze(1).to_broadcast([128, 8, 128]), op0=ALU.min, op1=ALU.mult),
                       [t2, cb], [MT])
                    for r in range(8):
                        op("pe", lambda e, r=r: e.matmul(py[:, r * 64:(r + 1) * 64], MT[:, r, :], xtok[:, c, r * 64:(r + 1) * 64],
                                                         start=True, stop=True), [MT, xtok], [py])
                    op("pool", lambda e: e.tensor_tensor(out=xd[:].rearrange("p (r c) -> p r c", c=64),
                                                         in0=xtok[:, c, :].rearrange("p (r c) -> p r c", c=64),
                                                         in1=dte[:, c, cs0:cs0 + 8].unsqueeze(2).to_broadcast([128, 8, 64]), op=ALU.mult), [xtok, dte], [xd])
                    op("pe", lambda e: e.matmul(pSt[dr][:], Btok[:, c, :], xd[:], start=True, stop=True), [Btok, xd], [pSt[dr]])

                def rec(dr, c):
                    cs0 = dr * 8
                    csl = slice(c * 128, (c + 1) * 128)
                    py = pYs[dr]
                    first = (dr == 0 and c < 8) or (dr == 1 and c >= 8)
                    op("pe", lambda e: e.matmul(pO[:], CTt[:, csl], hbf[dr][:], start=True, stop=True), [CTt, hbf[dr]], [pO])
                    op("dve", lambda e: e.tensor_tensor(out=hst[dr][:].rearrange("p (r c) -> p r c", c=64),
                                                        in0=hst[dr][:].rearrange("p (r c) -> p r c", c=64),
                                                        in1=etot[:, c, cs0:cs0 + 8].unsqueeze(2).to_broadcast([128, 8, 64]), op=ALU.mult), [hst[dr], etot], [hst[dr]])
                    op("dve", lambda e: e.tensor_tensor(out=hst[dr][:], in0=hst[dr][:], in1=pSt[dr][:], op=ALU.add), [hst[dr], pSt[dr]], [hst[dr]])
                    op("act", lambda e: e.copy(out=hbf[dr][:], in_=hst[dr][:]), [hst[dr]], [hbf[dr]])
                    op("dve", lambda e: e.tensor_tensor(out=tmp1[:].rearrange("p (r c) -> p r c", c=64),
                                                        in0=pO[:].rearrange("p (r c) -> p r c", c=64),
                                                        in1=eacs[:, c, cs0:cs0 + 8].unsqueeze(2).to_broadcast([128, 8, 64]), op=ALU.mult),
                       [pO, eacs], [tmp1])
                    if first:
                        op("dve", lambda e: e.tensor_tensor(out=yf[:, c, :], in0=tmp1[:], in1=py[:], op=ALU.add), [tmp1, py], [yf])
                    else:
                        op("dve", lambda e: e.tensor_tensor(out=ysum[:], in0=tmp1[:], in1=py[:], op=ALU.add), [tmp1, py], [ysum])
                        op("dve", lambda e: e.tensor_tensor(out=ysum[:], in0=ysum[:], in1=yf[:, c, :], op=ALU.add), [ysum, yf], [ysum])
                        op("pool", lambda e: e.tensor_tensor(out=dskx[:].rearrange("p (r c) -> p r c", c=64),
                                                             in0=xtok[:, c, :].rearrange("p (r c) -> p r c", c=64),
                                                             in1=dsk[:, g * 8:g * 8 + 8].unsqueeze(2).to_broadcast([128, 8, 64]), op=ALU.mult),
                           [xtok, dsk], [dskx])
                        op("dve", lambda e: e.tensor_tensor(out=yf[:, c, :], in0=ysum[:], in1=dskx[:], op=ALU.add), [ysum, dskx], [yf])

                def gate4(c0):
                    for i in range(4):
                        c = c0 + i
                        csl = slice(c * 128, (c + 1) * 128)
                        pz = pin[i % 2]
                        for k in range(8):
                            op("pe", lambda e, k=k, csl=csl, pz=pz: e.matmul(pz[:], hT[:, k, csl], wz[:, k, :], start=(k == 0), stop=(k == 7)), [hT, wz], [pz])
                        op("act", lambda e, i=i, pz=pz: e.activation(out=zs4[:, i, :], in_=pz[:], func=AF.Silu), [pz], [zs4])
                    for i in range(4):
                        c = c0 + i
                        op("dve", lambda e, i=i, c=c: e.tensor_tensor(out=ub[:], in0=yf[:, c, :], in1=zs4[:, i, :], op=ALU.mult), [yf, zs4], [ub])
                        op("act", lambda e, i=i: e.activation(out=zs[:], in_=ub[:], func=AF.Square, accum_out=gst4[:, i:i + 1]), [ub], [zs, gst4])
                    op("dve", lambda e: e.tensor_scalar(out=gst4[:, 4:8], in0=gst4[:, 0:4], scalar1=1.0 / 512, scalar2=EPS, op0=ALU.mult, op1=ALU.add), [gst4], [gst4])
                    op("act", lambda e: e.sqrt(out=gst4[:, 8:12], in_=gst4[:, 4:8]), [gst4], [gst4])
                    op("dve", lambda e: e.reciprocal(out=gst4[:, 12:16], in_=gst4[:, 8:12]), [gst4], [gst4])
                    for i in range(4):
                        c = c0 + i
                        csl = slice(c * 128, (c + 1) * 128)
                        op("dve", lambda e, i=i, c=c: e.tensor_tensor(out=ub[:], in0=yf[:, c, :], in1=zs4[:, i, :], op=ALU.mult), [yf, zs4], [ub])
                        op("dve", lambda e, i=i: e.scalar_tensor_tensor(out=yn16[:], in0=ub[:], scalar=gst4[:, 12 + i:13 + i], in1=ssdn[:],
                                                                        op0=ALU.mult, op1=ALU.mult), [ub, gst4, ssdn], [yn16])
                        for j in range(4):
                            op("pe", lambda e, j=j: e.transpose(ptr[:, j * 128:(j + 1) * 128], yn16[:, j * 128:(j + 1) * 128], ident16[:]), [yn16, ident16], [ptr])
                        op("act", lambda e, csl=csl: e.copy(out=ynT[:, :, csl], in_=ptr[:, 0:512].rearrange("p (j c) -> p j c", c=128)), [ptr], [ynT])

                pSt = (pin[1], ptr32)
                orders = (list(range(NT)), list(range(NT - 1, -1, -1)))
                for s_ in range(NT + 1):
                    for dr in range(2):
                        if s_ < NT:
                            prepA(dr, orders[dr][s_])
                        if s_ >= 1:
                            rec(dr, orders[dr][s_ - 1])
                        if s_ < NT:
                            prepB(dr, orders[dr][s_])
                for c0 in range(0, NT, 4):
                    gate4(c0)
                dma("sp", ynT_d[g * 4:(g + 1) * 4].rearrange("j p t -> p j t"), ynT[:], [ynT], [], ynT)
                if g == 0:
                    dump("ynT0", ynT[:], [ynT])
            for g in range(4):
                do_group(g)
            kb.barrier()
          kb.stack = G


        if stop_after >= 3 and 3 not in skip:
          with ExitStack() as P:
            kb.stack = P
            winv = I["w_in"].rearrange("(k p) n -> p k n", p=128)
            wqkpv = I["w_qkp"].rearrange("(k p) n -> p k n", p=128)
            cosT = kb.sb([128, T]); sinT = kb.sb([128, T]); mask = kb.sb([128, 64])
            dma("sp", cosT[:], I["cosT"], [], [cosT], cosT)
            dma("sp", sinT[:], I["sinT"], [], [sinT], sinT)
            dma("sp", mask[:], I["mask01"], [], [mask], mask)
            EB = kb.sb([128, 16, 2, 7, 64], BF16)
            rbs = kb.sb([128, 14, 64])
            rbv = I["rb"].rearrange("p (h d c) -> p h d c", h=16, d=14)
            for h in range(16):
                dma("sp", rbs[:], rbv[:, h], [], [rbs], rbs)
                op("act", lambda e: e.activation(out=rbs[:], in_=rbs[:], func=AF.Exp), [rbs], [rbs])
                op("dve", lambda e, h=h: e.tensor_tensor(out=EB[:, h].rearrange("p v u c -> p (v u) c"), in0=rbs[:],
                                                         in1=mask[:].unsqueeze(1).to_broadcast([128, 14, 64]), op=ALU.mult), [rbs, mask], [EB])
            wq = kb.sb([128, 8, 128], BF16); wk = kb.sb([128, 8, 128], BF16); wv = kb.sb([128, 8, 128], BF16)
            wqp = kb.sb([128, 8, 128], BF16); wkp = kb.sb([128, 8, 128], BF16)
            qT = kb.sb([128, T], BF16); kT = kb.sb([128, T], BF16); kcT = kb.sb([128, CT], BF16)
            qz = [kb.sb([128, T], BF16) for _ in range(2)]
            for zz in qz:
                op("dve", lambda e, zz=zz: e.memset(zz[:], 0.0), [], [zz])
            vE = kb.sb([128, 16, 256], BF16); vO = kb.sb([128, 15, 256], BF16); vC = kb.sb([128, 2, 256], BF16)
            for vv in (vE, vO, vC):
                op("dve", lambda e, vv=vv: e.memset(vv[:], 1.0), [], [vv])
            onaT = kb.sb([128, T], BF16)
            r1 = kb.sb([128, 512]); r2 = kb.sb([128, 512])
            PT = [kb.sb([128, 768], BF16) for _ in range(2)]
            rec = [kb.sb([128, 128]) for _ in range(2)]
            bA = kb.ps([128, 512]); bB = kb.ps([128, 512]); bC = kb.ps([128, 512])
            pSs = [kb.ps([128, 1024]) for _ in range(2)]
            pOD = [bA, bB]

            def rope_proj(w, wp, dst):
                for tb in range(4):
                    ts = slice(tb * 512, (tb + 1) * 512)
                    for k in range(8):
                        op("pe", lambda e, k=k, ts=ts: e.matmul(bA[:], w[:, k, :], hT[:, k, ts], start=(k == 0), stop=(k == 7)), [w, hT], [bA])
                    for k in range(8):
                        op("pe", lambda e, k=k, ts=ts: e.matmul(bB[:], wp[:, k, :], hT[:, k, ts], start=(k == 0), stop=(k == 7)), [wp, hT], [bB])
                    op("dve", lambda e, ts=ts: e.tensor_tensor(out=r1[:], in0=bA[:], in1=cosT[:, ts], op=ALU.mult), [bA, cosT], [r1])
                    op("dve", lambda e, ts=ts: e.tensor_tensor(out=r2[:], in0=bB[:], in1=sinT[:, ts], op=ALU.mult), [bB, sinT], [r2])
                    op("dve", lambda e, ts=ts: e.tensor_tensor(out=dst[:, ts], in0=r1[:], in1=r2[:], op=ALU.add), [r1, r2], [dst])

            def v_tiles(dstv, ntile, src, tok0):
                for i0 in range(0, ntile, 4):
                    n = min(4, ntile - i0)
                    for ii in range(n):
                        c0 = tok0 + (i0 + ii) * 128
                        for k in range(8):
                            op("pe", lambda e, k=k, ii=ii, c0=c0: e.matmul(bC[:, ii * 128:(ii + 1) * 128], src[:, k, c0:c0 + 128], wv[:, k, :],
                                                                         start=(k == 0), stop=(k == 7)), [src, wv], [bC])
                    pv = bC[:, 0:n * 128].rearrange("p (i c) -> p i c", c=128)
                    op("act", lambda e, i0=i0, n=n, pv=pv: e.copy(out=dstv[:, i0:i0 + n, 64:192], in_=pv[:, :, 0:128]), [bC], [dstv])

            def row_scores(hp, r):
                s = r % 2
                pS = pSs[s]; pt = PT[s]
                r0 = min(max(r - 4, 0), 24)
                base = r0 - r + 7
                v_, u0 = base % 2, base // 2
                qs = slice(r * 64, (r + 1) * 64)
                for h in range(2):
                    hs = slice(h * 64, (h + 1) * 64)
                    for m in range(6):
                        if m < 4:
                            ks = (r0 + 2 * m) * 64
                            lhs = kT[:, ks:ks + 128]
                        else:
                            lhs = kcT[:, (m - 4) * 128:(m - 3) * 128]
                        o0 = h * 384 + m * 64
                        op("pe", lambda e, lhs=lhs, o0=o0, h=h: e.matmul(pS[:, o0:o0 + 64], lhs, qz[h][:, qs], start=True, stop=True), [kT, kcT, qz[h]], [pS])
                op("act", lambda e: e.activation(out=pt[:, 0:512], in_=pS[:, 0:512], func=AF.Exp, scale=0.125), [pS], [pt])
                op("act", lambda e: e.activation(out=pt[:, 512:768], in_=pS[:, 512:768], func=AF.Exp, scale=0.125), [pS], [pt])
                ptv = pt[:].rearrange("p (h m c) -> p h m c", h=2, m=6)
                op("dve", lambda e: e.tensor_tensor(out=ptv[:, :, 0:4, :], in0=ptv[:, :, 0:4, :], in1=EB[:, 2 * hp:2 * hp + 2, v_, u0:u0 + 4, :], op=ALU.mult),
                   [pt, EB], [pt])

            def row_pv(hp, r):
                s = r % 2
                pt = PT[s]; po = pOD[s]; rc = rec[s]
                r0 = min(max(r - 4, 0), 24)
                qs = slice(r * 64, (r + 1) * 64)
                ptv = pt[:].rearrange("p (h m c) -> p h m c", h=2, m=6)
                for h in range(2):
                    for m in range(6):
                        if m < 4:
                            kr = r0 + 2 * m
                            vt = vE[:, kr // 2, h * 128:(h + 1) * 128] if kr % 2 == 0 else vO[:, (kr - 1) // 2, h * 128:(h + 1) * 128]
                        else:
                            vt = vC[:, m - 4, h * 128:(h + 1) * 128]
                        op("pe", lambda e, vt=vt, h=h, m=m: e.matmul(po[:, h * 64:(h + 1) * 64], vt, ptv[:, h, m, :], start=(m == 0), stop=(m == 5)),
                           [vE, vO, vC, pt], [po])
                op("dve", lambda e: e.reciprocal(out=rc[0:64, 0:64], in_=po[0:64, 0:64]), [po], [rc])
                op("dve", lambda e: e.reciprocal(out=rc[64:128, 64:128], in_=po[64:128, 64:128]), [po], [rc])
                op("dve", lambda e: e.tensor_tensor(out=onaT[0:64, qs], in0=po[64:128, 0:64], in1=rc[0:64, 0:64], op=ALU.mult), [po, rc], [onaT])
                op("dve", lambda e: e.tensor_tensor(out=onaT[64:128, qs], in0=po[0:64, 64:128], in1=rc[64:128, 64:128], op=ALU.mult), [po, rc], [onaT])

            def do_pair(hp):
                c0 = hp * 128
                dma("pool", wq[:], winv[:, :, c0:c0 + 128], [], [wq], wq)
                dma("pool", wk[:], winv[:, :, 1024 + c0:1024 + c0 + 128], [], [wk], wk)
                dma("pool", wv[:], winv[:, :, 2048 + c0:2048 + c0 + 128], [], [wv], wv)
                dma("pool", wqp[:], wqkpv[:, :, c0:c0 + 128], [], [wqp], wqp)
                dma("pool", wkp[:], wqkpv[:, :, 1024 + c0:1024 + c0 + 128], [], [wkp], wkp)
                rope_proj(wq, wqp, qT)
                rope_proj(wk, wkp, kT)
                op("act", lambda e: e.copy(out=qz[0][0:64, :], in_=qT[0:64, :]), [qT], [qz[0]])
                op("act", lambda e: e.copy(out=qz[1][64:128, :], in_=qT[64:128, :]), [qT], [qz[1]])
                for k in range(8):
                    op("pe", lambda e, k=k: e.matmul(bA[:, 0:CT], wk[:, k, :], hcT[:, k, :], start=(k == 0), stop=(k == 7)), [wk, hcT], [bA])
                op("act", lambda e: e.copy(out=kcT[:], in_=bA[:, 0:CT]), [bA], [kcT])
                v_tiles(vE, 16, hT, 0)
                v_tiles(vO, 15, hT, 64)
                v_tiles(vC, 2, hcT, 0)
                row_scores(hp, 0)
                for r in range(32):
                    if r + 1 < 32:
                        row_scores(hp, r + 1)
                    row_pv(hp, r)
                dma("sp", onaT_d[hp], onaT[:], [onaT], [], onaT)
                if hp == 0:
                    dump("qT0", qT[:], [qT]); dump("kT0", kT[:], [kT]); dump("onaT0", onaT[:], [onaT])

            for hp in range(8):
                do_pair(hp)
            kb.barrier()
          kb.stack = G


        kb.stack = G
        modbc = kb.sb([128, 4, 1024])
        affs = kb.sb([128, NT, 16])
        if stop_after >= 4 and 4 not in skip:
          with ExitStack() as P0:
            kb.stack = P0
            cc2 = kb.sb([128, 16]); sc32 = kb.sb([128, 16]); screp = kb.sb([128, 8, 128], BF16)
            vecb = kb.sb([128, 3072]); wa2 = kb.sb([128, 8, 1024], BF16); badb = kb.sb([128, 1024])
            pm = [kb.ps([128, 512]) for _ in range(2)]
            dma("sp", cc2[:], I["cc"], [], [cc2], cc2)
            dma("sp", vecb[:], I["vec_bc"], [], [vecb], vecb)
            op("act", lambda e: e.activation(out=sc32[:], in_=cc2[:], func=AF.Silu), [cc2], [sc32])
            op("dve", lambda e: e.tensor_copy(out=screp[:], in_=sc32[:].rearrange("p (k w) -> p k w", w=2)[:, :, 0:1].to_broadcast([128, 8, 128])),
               [sc32], [screp])
            wav2 = I["w_ada"].rearrange("(k p) n -> p k n", p=128)
            for c in range(4):
                dma("pool", wa2[:], wav2[:, :, (c + 2) * 1024:(c + 3) * 1024], [], [wa2], wa2)
                dma("sp", badb[:], I["b_ada_bc"][:, c * 1024:(c + 1) * 1024], [], [badb], badb)
                for hf in range(2):
                    for k in range(8):
                        op("pe", lambda e, k=k, hf=hf: e.matmul(pm[hf][:], screp[:, k, :], wa2[:, k, hf * 512:(hf + 1) * 512], start=(k == 0), stop=(k == 7)),
                           [screp, wa2], [pm[hf]])
                    op("dve", lambda e, c=c, hf=hf: e.tensor_tensor(out=modbc[:, c, hf * 512:(hf + 1) * 512], in0=pm[hf][:], in1=badb[:, hf * 512:(hf + 1) * 512], op=ALU.add),
                       [pm[hf], badb], [modbc])
            op("dve", lambda e: e.tensor_tensor(out=modbc[:, 0, :], in0=modbc[:, 0, :], in1=vecb[:, 0:1024], op=ALU.mult), [modbc, vecb], [modbc])
            op("dve", lambda e: e.scalar_tensor_tensor(out=modbc[:, 2, :], in0=modbc[:, 2, :], scalar=1.0, in1=vecb[:, 1024:2048], op0=ALU.add, op1=ALU.mult),
               [modbc, vecb], [modbc])
            op("dve", lambda e: e.tensor_tensor(out=modbc[:, 3, :], in0=modbc[:, 3, :], in1=vecb[:, 2048:3072], op=ALU.mult), [modbc, vecb], [modbc])
            kb.barrier()
          with ExitStack() as P:
            kb.stack = P
            winv = I["w_in"].rearrange("(k p) n -> p k n", p=128)
            wbnv = I["w_bna"].rearrange("(k p) n -> p k n", p=128)
            wbsv = I["w_bssd"].rearrange("(k p) n -> p k n", p=128)
            wo = kb.sb([128, 8, 1024], BF16)
            dma("pool", wo[:], I["w_out"].rearrange("(k p) n -> p k n", p=128), [], [wo], wo)
            wr = kb.sb([128, 8, 16])
            dma("sp", wr[:], I["w_router"].rearrange("(k p) n -> p k n", p=128), [], [wr], wr)
            onaTs = kb.sb([128, 8, 1024], BF16); ynTs = kb.sb([128, 16, 1024], BF16); uT = kb.sb([128, 8, 1024], BF16)
            wbn = [kb.sb([128, 8, 128], BF16) for _ in range(2)]; wbs = [kb.sb([128, 16, 128], BF16) for _ in range(2)]
            wg1 = [kb.sb([128, 8, 128], BF16) for _ in range(2)]; wg2 = [kb.sb([128, 8, 128], BF16) for _ in range(2)]
            s1 = kb.sb([128, 512]); s2 = kb.sb([128, 512])
            xt4 = kb.sb([128, D]); x1t4 = kb.sb([128, D]); tmpf = kb.sb([128, D]); h2f = kb.sb([128, D]); h2b = kb.sb([128, D], BF16)
            h2T = kb.sb([128, 8, 128]); stt4 = kb.sb([128, 8]); junk4 = kb.sb([128, D], BF16)
            lg = kb.sb([128, 16]); sm = kb.sb([128, 4])
            pA = kb.ps([128, 512]); pB = kb.ps([128, 512]); pG1 = kb.ps([128, 512]); pG2 = kb.ps([128, 512])
            pM = [kb.ps([128, 512]) for _ in range(2)]; pT = [kb.ps([128, 512]) for _ in range(2)]
            xv = I["x"].rearrange("(i p) d -> i p d", p=128)
            x1v = x1_d.rearrange("(i p) d -> i p d", p=128)
            h2v = h2_d.rearrange("(i p) d -> i p d", p=128)

            def merge_block(th, dc, tb, W):
                wbn_, wbs_, wg1_, wg2_ = W
                tsl = slice(tb * 512, (tb + 1) * 512)
                gsl = slice(th * 1024 + tb * 512, th * 1024 + (tb + 1) * 512)
                for k in range(8):
                    op("pe", lambda e, k=k: e.matmul(pA[:], wbn_[:, k, :], onaTs[:, k, tsl], start=(k == 0), stop=(k == 7)), [wbn_, onaTs], [pA])
                for k in range(16):
                    op("pe", lambda e, k=k: e.matmul(pB[:], wbs_[:, k, :], ynTs[:, k, tsl], start=(k == 0), stop=(k == 15)), [wbs_, ynTs], [pB])
                for k in range(8):
                    op("pe", lambda e, k=k: e.matmul(pG1[:], wg1_[:, k, :], hT[:, k, gsl], start=(k == 0), stop=(k == 7)), [wg1_, hT], [pG1])
                for k in range(8):
                    op("pe", lambda e, k=k: e.matmul(pG2[:], wg2_[:, k, :], hT[:, k, gsl], start=(k == 0), stop=(k == 7)), [wg2_, hT], [pG2])
                op("act", lambda e: e.activation(out=s1[:], in_=pG1[:], func=AF.Sigmoid), [pG1], [s1])
                op("act", lambda e: e.activation(out=s2[:], in_=pG2[:], func=AF.Sigmoid), [pG2], [s2])
                op("dve", lambda e: e.tensor_tensor(out=s1[:], in0=s1[:], in1=pA[:], op=ALU.mult), [s1, pA], [s1])
                op("dve", lambda e: e.tensor_tensor(out=s2[:], in0=s2[:], in1=pB[:], op=ALU.mult), [s2, pB], [s2])
                op("dve", lambda e: e.tensor_tensor(out=uT[:, dc, tsl], in0=s1[:], in1=s2[:], op=ALU.add), [s1, s2], [uT])

            def rstd_chain(col):
                op("dve", lambda e: e.tensor_scalar(out=stt4[:, col + 1:col + 2], in0=stt4[:, col:col + 1], scalar1=1.0 / D, scalar2=EPS, op0=ALU.mult, op1=ALU.add), [stt4], [stt4])
                op("act", lambda e: e.sqrt(out=stt4[:, col + 2:col + 3], in_=stt4[:, col + 1:col + 2]), [stt4], [stt4])
                op("dve", lambda e: e.reciprocal(out=stt4[:, col + 3:col + 4], in_=stt4[:, col + 2:col + 3]), [stt4], [stt4])

            def post_tile(th, i):
                gi = th * 8 + i
                isl = slice(i * 128, (i + 1) * 128)
                for hf in range(2):
                    for k in range(8):
                        op("pe", lambda e, k=k, hf=hf: e.matmul(pM[hf][:], uT[:, k, isl], wo[:, k, hf * 512:(hf + 1) * 512], start=(k == 0), stop=(k == 7)), [uT, wo], [pM[hf]])
                dma("sp", xt4[:], xv[gi], [], [xt4], xt4)
                op("act", lambda e: e.activation(out=junk4[:, 0:512], in_=pM[0][:], func=AF.Square, accum_out=stt4[:, 0:1]), [pM[0]], [junk4, stt4])
                op("act", lambda e: e.activation(out=junk4[:, 512:1024], in_=pM[1][:], func=AF.Square, accum_out=stt4[:, 4:5]), [pM[1]], [junk4, stt4])
                op("dve", lambda e: e.tensor_tensor(out=stt4[:, 0:1], in0=stt4[:, 0:1], in1=stt4[:, 4:5], op=ALU.add), [stt4], [stt4])
                rstd_chain(0)
                for hf in range(2):
                    hs = slice(hf * 512, (hf + 1) * 512)
                    op("dve", lambda e, hf=hf, hs=hs: e.scalar_tensor_tensor(out=x1t4[:, hs], in0=pM[hf][:], scalar=stt4[:, 3:4], in1=modbc[:, 0, hs], op0=ALU.mult, op1=ALU.mult),
                       [pM[hf], stt4, modbc], [x1t4])
                op("dve", lambda e: e.tensor_tensor(out=x1t4[:], in0=x1t4[:], in1=xt4[:], op=ALU.add), [x1t4, xt4], [x1t4])
                dma("sp", x1v[gi], x1t4[:], [x1t4], [], x1t4)
                if gi == 0:
                    dump("x1_0", x1t4[:], [x1t4])
                op("act", lambda e: e.activation(out=junk4[:], in_=x1t4[:], func=AF.Square, accum_out=stt4[:, 0:1]), [x1t4], [junk4, stt4])
                rstd_chain(0)
                op("dve", lambda e: e.scalar_tensor_tensor(out=tmpf[:], in0=x1t4[:], scalar=stt4[:, 3:4], in1=modbc[:, 2, :], op0=ALU.mult, op1=ALU.mult), [x1t4, stt4, modbc], [tmpf])
                op("dve", lambda e: e.tensor_tensor(out=h2f[:], in0=tmpf[:], in1=modbc[:, 1, :], op=ALU.add), [tmpf, modbc], [h2f])
                op("act", lambda e: e.copy(out=h2b[:], in_=h2f[:]), [h2f], [h2b])
                dma("sp", h2v[gi], h2b[:], [h2b], [], h2b)
                for k in range(8):
                    op("pe", lambda e, k=k: e.transpose(pT[k // 4][:, (k % 4) * 128:(k % 4 + 1) * 128], h2f[:, k * 128:(k + 1) * 128], ident32[:]), [h2f, ident32], [pT[k // 4]])
                op("act", lambda e: e.copy(out=h2T[:, 0:4, :], in_=pT[0][:].rearrange("p (k c) -> p k c", c=128)), [pT[0]], [h2T])
                op("dve", lambda e: e.tensor_copy(out=h2T[:, 4:8, :], in_=pT[1][:].rearrange("p (k c) -> p k c", c=128)), [pT[1]], [h2T])
                for k in range(8):
                    op("pe", lambda e, k=k: e.matmul(pA[:, 0:16], h2T[:, k, :], wr[:, k, :], start=(k == 0), stop=(k == 7)), [h2T, wr], [pA])
                op("dve", lambda e: e.tensor_copy(out=lg[:], in_=pA[:, 0:16]), [pA], [lg])
                op("dve", lambda e: e.reduce_max(out=sm[:, 0:1], in_=lg[:], axis=AX.X), [lg], [sm])
                op("dve", lambda e: e.tensor_scalar(out=sm[:, 1:2], in0=sm[:, 0:1], scalar1=-1.0, scalar2=None, op0=ALU.mult), [sm], [sm])
                op("act", lambda e: e.activation(out=lg[:], in_=lg[:], func=AF.Exp, bias=sm[:, 1:2], accum_out=sm[:, 2:3]), [lg, sm], [lg, sm])
                op("dve", lambda e: e.reciprocal(out=sm[:, 3:4], in_=sm[:, 2:3]), [sm], [sm])
                op("dve", lambda e: e.tensor_scalar(out=affs[:, gi, :], in0=lg[:], scalar1=sm[:, 3:4], scalar2=None, op0=ALU.mult), [lg, sm], [affs])

            for th in range(2):
                hsl = slice(th * 1024, (th + 1) * 1024)
                dma("sp", onaTs[:], onaT_d[:, :, hsl].rearrange("c p t -> p c t"), [], [onaTs], onaTs)
                dma("sp", ynTs[:], ynT_d[:, :, hsl].rearrange("c p t -> p c t"), [], [ynTs], ynTs)
                for dc in range(8):
                    s_ = dc % 2
                    W = (wbn[s_], wbs[s_], wg1[s_], wg2[s_])
                    dsl = slice(dc * 128, (dc + 1) * 128)
                    dma("pool", W[0][:], wbnv[:, :, dsl], [], [W[0]], W[0])
                    dma("pool", W[1][:], wbsv[:, :, dsl], [], [W[1]], W[1])
                    dma("pool", W[2][:], winv[:, :, 8256 + dc * 128:8256 + (dc + 1) * 128], [], [W[2]], W[2])
                    dma("pool", W[3][:], winv[:, :, 9280 + dc * 128:9280 + (dc + 1) * 128], [], [W[3]], W[3])
                    for tb in range(2):
                        merge_block(th, dc, tb, W)
                for i in range(8):
                    post_tile(th, i)
            dump("affs", affs[:], [affs])
            kb.barrier()
          kb.stack = G

        if stop_after >= 5 and 5 not in skip:
          with ExitStack() as P:
            kb.stack = P
            h2 = kb.sb([128, NT, D], BF16)
            dma("sp", h2[:], h2_d.rearrange("(i p) d -> p i d", p=128), [], [h2], h2)
            yo = kb.sb([128, 32, D], BF16)
            csm16 = kb.sb([16, T], BF16); csmT = kb.sb([128, NT, 16]); affhl = kb.sb([128, NT, 16, 2], BF16)
            iota1 = kb.sb([128, 256]); slotp1 = kb.sb([128, 2]); sel16 = kb.sb([16, 16, 128], BF16)
            dma("sp", iota1[:], I["iota1"], [], [iota1], iota1)
            dma("sp", slotp1[:], I["slotp1"], [], [slotp1], slotp1)
            dma("pool", sel16[:], I["sel16"].rearrange("k (e m) -> k e m", e=16), [], [sel16], sel16)
            with ExitStack() as P1:
                kb.stack = P1
                affT = kb.sb([16, T]); work = kb.sb([16, T]); csb = kb.sb([16, T]); mx8 = kb.sb([16, 8]); onesr = kb.sb([16, T])
                hi32 = kb.sb([128, NT, 16]); lo32 = kb.sb([128, NT, 16])
                pr = [kb.ps([128, 512]) for _ in range(2)]
                op("dve", lambda e: e.tensor_copy(out=affhl[:, :, :, 0], in_=affs[:]), [affs], [affhl])
                op("dve", lambda e: e.tensor_copy(out=hi32[:], in_=affhl[:, :, :, 0]), [affhl], [hi32])
                op("dve", lambda e: e.tensor_tensor(out=lo32[:], in0=affs[:], in1=hi32[:], op=ALU.subtract), [affs, hi32], [lo32])
                op("dve", lambda e: e.tensor_copy(out=affhl[:, :, :, 1], in_=lo32[:]), [lo32], [affhl])
                for i in range(NT):
                    pp = pr[(i // 4) % 2]
                    op("pe", lambda e, i=i, pp=pp: e.transpose(pp[0:16, (i % 4) * 128:(i % 4 + 1) * 128], affs[:, i, :], ident32[:]), [affs, ident32], [pp])
                    if i % 4 == 3:
                        op("dve", lambda e, i=i, pp=pp: e.tensor_copy(out=affT[:, (i - 3) * 128:(i + 1) * 128], in_=pp[0:16, :]), [pp], [affT])
                op("dve", lambda e: e.tensor_copy(out=work[:], in_=affT[:]), [affT], [work])
                for rnd in range(32):
                    op("dve", lambda e: e.max(out=mx8[:], in_=work[:]), [work], [mx8])
                    if rnd < 31:
                        op("dve", lambda e: e.match_replace(out=work[:], in_to_replace=mx8[:], in_values=work[:], imm_value=-1e30), [mx8, work], [work])
                op("dve", lambda e: e.tensor_scalar(out=work[:], in0=affT[:], scalar1=mx8[:, 7:8], scalar2=None, op0=ALU.is_ge), [affT, mx8], [work])
                op("dve", lambda e: e.memset(onesr[:], 1.0), [], [onesr])
                op("dve", lambda e: e.tensor_tensor_scan(out=csb[:], data0=onesr[:], data1=work[:], initial=0.0, op0=ALU.mult, op1=ALU.add), [onesr, work], [csb])
                op("dve", lambda e: e.tensor_tensor(out=csb[:], in0=csb[:], in1=work[:], op=ALU.mult), [csb, work], [csb])
                op("dve", lambda e: e.tensor_copy(out=csm16[:], in_=csb[:]), [csb], [csm16])
                for i in range(NT):
                    pp = pr[(i // 8) % 2]
                    op("pe", lambda e, i=i, pp=pp: e.transpose(pp[:, (i % 8) * 16:(i % 8 + 1) * 16], csb[:, i * 128:(i + 1) * 128], ident32[0:16, 0:16]), [csb, ident32], [pp])
                    if i % 8 == 7:
                        op("dve", lambda e, i=i, pp=pp: e.tensor_copy(out=csmT[:, i - 7:i + 1, :], in_=pp[:, 0:128].rearrange("p (i c) -> p i c", c=16)), [pp], [csmT])
                dump("csm", csb[:], [csb])
                kb.barrier()
            kb.stack = P
            with ExitStack() as P2:
                kb.stack = P2
                Se = kb.sb([128, NT, 256], BF16); xg = kb.sb([128, 8, 256], BF16); hid = kb.sb([128, 16, 256], BF16)
                weg = [kb.sb([128, 8, 256], BF16) for _ in range(2)]; weu = [kb.sb([128, 8, 256], BF16) for _ in range(2)]
                wed = [kb.sb([128, 2, D], BF16) for _ in range(2)]
                sgs = [kb.sb([128, 256]) for _ in range(2)]; gt = kb.sb([128, 4])
                pX = kb.ps([128, 512]); pGa = kb.ps([128, 512]); pUp = kb.ps([128, 512]); pUp2 = kb.ps([128, 512]); pDn = [kb.ps([128, 512]) for _ in range(4)]
                pGas = (pGa, pX); pUps = (pUp, pUp2)

                def do_expert(e_):
                    for i in range(NT):
                        op("dve", lambda e, i=i: e.tensor_scalar(out=Se[:, i, :], in0=iota1[:], scalar1=csmT[:, i, e_:e_ + 1], scalar2=None, op0=ALU.is_equal), [iota1, csmT], [Se])
                    for dk in range(8):
                        for i in range(NT):
                            op("pe", lambda e, i=i, dk=dk: e.matmul(pX[:, 0:256], h2[:, i, dk * 128:(dk + 1) * 128], Se[:, i, :], start=(i == 0), stop=(i == NT - 1)), [h2, Se], [pX])
                        if dk % 2 == 0:
                            op("act", lambda e, dk=dk: e.copy(out=xg[:, dk, :], in_=pX[:, 0:256]), [pX], [xg])
                        else:
                            op("dve", lambda e, dk=dk: e.tensor_copy(out=xg[:, dk, :], in_=pX[:, 0:256]), [pX], [xg])
                    for sc in range(2):
                        for i in range(NT):
                            op("pe", lambda e, i=i, sc=sc: e.matmul(pX[:, 256 + sc * 2:258 + sc * 2], Se[:, i, sc * 128:(sc + 1) * 128], affhl[:, i, e_, :], start=(i == 0), stop=(i == NT - 1)),
                               [Se, affhl], [pX])
                    op("dve", lambda e: e.tensor_copy(out=gt[:], in_=pX[:, 256:260]), [pX], [gt])
                    op("dve", lambda e: e.tensor_tensor(out=gt[:, 0:1], in0=gt[:, 0:1], in1=gt[:, 1:2], op=ALU.add), [gt], [gt])
                    op("dve", lambda e: e.tensor_tensor(out=gt[:, 1:2], in0=gt[:, 2:3], in1=gt[:, 3:4], op=ALU.add), [gt], [gt])
                    def load_w(fb):
                        s_ = (e_ * 8 + fb) % 2
                        fsl = slice(fb * 256, (fb + 1) * 256)
                        dma("pool", weg[s_][:], I["w_eg"][e_].rearrange("(k p) f -> p k f", p=128)[:, :, fsl], [], [weg[s_]], weg[s_])
                        dma("pool", weu[s_][:], I["w_eu"][e_].rearrange("(k p) f -> p k f", p=128)[:, :, fsl], [], [weu[s_]], weu[s_])
                        dma("pool", wed[s_][:], I["w_ed"][e_][fsl, :].rearrange("(c p) d -> p c d", p=128), [], [wed[s_]], wed[s_])

                    def gateup(fch):
                        fb, fc = fch // 2, fch % 2
                        s_ = (e_ * 8 + fb) % 2
                        pg = pGas[fch % 2]; pu = pUps[fch % 2]; sg_ = sgs[fch % 2]
                        for k in range(8):
                            op("pe", lambda e, k=k: e.matmul(pg[:, 0:256], weg[s_][:, k, fc * 128:(fc + 1) * 128], xg[:, k, :], start=(k == 0), stop=(k == 7)), [weg[s_], xg], [pg])
                        for k in range(8):
                            op("pe", lambda e, k=k: e.matmul(pu[:, 0:256], weu[s_][:, k, fc * 128:(fc + 1) * 128], xg[:, k, :], start=(k == 0), stop=(k == 7)), [weu[s_], xg], [pu])
                        op("act", lambda e: e.activation(out=sg_[:], in_=pg[:, 0:256], func=AF.Silu), [pg], [sg_])
                        op("dve", lambda e: e.tensor_tensor(out=hid[:, fch, :], in0=sg_[:], in1=pu[:, 0:256], op=ALU.mult), [sg_, pu], [hid])

                    def down(fch):
                        fb, fc = fch // 2, fch % 2
                        s_ = (e_ * 8 + fb) % 2
                        for sc in range(2):
                            for dh in range(2):
                                op("pe", lambda e, sc=sc, dh=dh: e.matmul(pDn[sc * 2 + dh][:], hid[:, fch, sc * 128:(sc + 1) * 128], wed[s_][:, fc, dh * 512:(dh + 1) * 512],
                                                                         start=(fch == 0), stop=(fch == 15)), [hid, wed[s_]], [pDn[sc * 2 + dh]])

                    for fch in range(16):
                        if fch % 2 == 0:
                            load_w(fch // 2)
                        gateup(fch)
                        if fch >= 1:
                            down(fch - 1)
                    down(15)
                    for sc in range(2):
                        for dh in range(2):
                            op("act", lambda e, sc=sc, dh=dh: e.activation(out=yo[:, e_ * 2 + sc, dh * 512:(dh + 1) * 512], in_=pDn[sc * 2 + dh][:], func=AF.Copy, scale=gt[:, sc:sc + 1]),
                               [pDn[sc * 2 + dh], gt], [yo])

                for e_ in range(16):
                    do_expert(e_)
                kb.barrier()
            kb.stack = P
            with ExitStack() as P3:
                kb.stack = P3
                ST = kb.sb([128, 16, 2, 128], BF16); x1t5 = kb.sb([128, D]); ot = kb.sb([128, D]); junk5 = kb.sb([128, D], BF16); stt5 = kb.sb([128, 8])
                pbc = [kb.ps([128, 512]) for _ in range(4)]; pF = [kb.ps([128, 512]) for _ in range(2)]
                x1v = x1_d.rearrange("(i p) d -> i p d", p=128)
                outv = OUT.rearrange("(i p) d -> i p d", p=128)

                def final_tile(i):
                    isl = slice(i * 128, (i + 1) * 128)
                    dma("sp", x1t5[:], x1v[i], [], [x1t5], x1t5)
                    for e_ in range(16):
                        op("pe", lambda e, e_=e_: e.matmul(pbc[e_ // 4][:, (e_ % 4) * 128:(e_ % 4 + 1) * 128], sel16[:, e_, :], csm16[:, isl], start=True, stop=True), [sel16, csm16], [pbc[e_ // 4]])
                    for b4 in range(4):
                        for sc in range(2):
                            op("dve", lambda e, b4=b4, sc=sc: e.tensor_scalar(out=ST[:, b4 * 4:(b4 + 1) * 4, sc, :], in0=pbc[b4][:].rearrange("p (e c) -> p e c", c=128),
                                                                               scalar1=slotp1[:, sc:sc + 1], scalar2=None, op0=ALU.is_equal), [pbc[b4], slotp1], [ST])
                    for dh in range(2):
                        n = 0
                        for e_ in range(16):
                            for sc in range(2):
                                op("pe", lambda e, e_=e_, sc=sc, dh=dh, n=n: e.matmul(pF[dh][:], ST[:, e_, sc, :], yo[:, e_ * 2 + sc, dh * 512:(dh + 1) * 512], start=(n == 0), stop=(n == 31)),
                                   [ST, yo], [pF[dh]])
                                n += 1
                    op("act", lambda e: e.activation(out=junk5[:, 0:512], in_=pF[0][:], func=AF.Square, accum_out=stt5[:, 0:1]), [pF[0]], [junk5, stt5])
                    op("act", lambda e: e.activation(out=junk5[:, 512:1024], in_=pF[1][:], func=AF.Square, accum_out=stt5[:, 4:5]), [pF[1]], [junk5, stt5])
                    op("dve", lambda e: e.tensor_tensor(out=stt5[:, 0:1], in0=stt5[:, 0:1], in1=stt5[:, 4:5], op=ALU.add), [stt5], [stt5])
                    op("dve", lambda e: e.tensor_scalar(out=stt5[:, 1:2], in0=stt5[:, 0:1], scalar1=1.0 / D, scalar2=EPS, op0=ALU.mult, op1=ALU.add), [stt5], [stt5])
                    op("act", lambda e: e.sqrt(out=stt5[:, 2:3], in_=stt5[:, 1:2]), [stt5], [stt5])
                    op("dve", lambda e: e.reciprocal(out=stt5[:, 3:4], in_=stt5[:, 2:3]), [stt5], [stt5])
                    for dh in range(2):
                        hs = slice(dh * 512, (dh + 1) * 512)
                        op("dve", lambda e, dh=dh, hs=hs: e.scalar_tensor_tensor(out=ot[:, hs], in0=pF[dh][:], scalar=stt5[:, 3:4], in1=modbc[:, 3, hs], op0=ALU.mult, op1=ALU.mult),
                           [pF[dh], stt5, modbc], [ot])
                    op("dve", lambda e: e.tensor_tensor(out=ot[:], in0=ot[:], in1=x1t5[:], op=ALU.add), [ot, x1t5], [ot])
                    dma("sp", outv[i], ot[:], [ot], [], ot)

                for i in range(NT):
                    final_tile(i)
                kb.barrier()
            kb.stack = P
          kb.stack = G

        if stop_after >= 99:
            pass
        kb.barrier()
        kb.emit()
    return nc


def kernel(**inputs):
    inp = {k: np.asarray(v, dtype=np.float32) for k, v in inputs.items()}
    shared = _host_prep(inp)
    import os
    nc = build_program(stop_after=int(os.environ.get("MK_STOP", "99")))
    nb = inp["x"].shape[0]
    in_maps = []
    for b in range(nb):
        m = dict(shared)
        m["x"] = np.ascontiguousarray(inp["x"][b])
        m["ctx"] = np.ascontiguousarray(inp["ctx"][b])
        cc = np.stack([inp["c"][b].reshape(8, 128).T, inp["c_ctx"].reshape(8, 128).T], axis=2).reshape(128, 16)
        m["cc"] = np.ascontiguousarray(cc)
        in_maps.append(m)
    res = run_bass_kernel_spmd(nc, in_maps, core_ids=list(range(nb)))
    out = np.stack([np.asarray(res.results[b]["out"]) for b in range(nb)], axis=0)
    return out.astype(np.float32)
```
